# Optimizing a Trainium2 kernel written in Bass

```python
import jax
import jax.numpy as jnp
from jax import lax

D_MODEL = 1024
BATCH = 2
SEQ = 16384
DEPTH = 2

GRID_W = 64
CTX_LEN = 256
EPS = 1e-6

CONV_DIM = 512
CONV_WIDTH = 3

HGRN_DIM = 512
HGRN_EXPAND = 128
HGRN_HEADS = HGRN_DIM // HGRN_EXPAND
HGRN_CHUNK = 64

ATT_HEADS = 8
ATT_KV_HEADS = 2
ATT_GROUP = ATT_HEADS // ATT_KV_HEADS
HEAD_DIM = 64
ATT_DIM = ATT_HEADS * HEAD_DIM
KV_DIM = ATT_KV_HEADS * HEAD_DIM
Q_BLOCK = 128
ROPE_THETA = 10000.0
ROPE_AXIS_DIM = HEAD_DIM // 2

N_BRANCH = 3
IN_NAMES = ('a_val', 'a_b', 'a_c', 'b_q', 'b_f_fwd', 'b_f_bwd', 'b_i', 'b_g', 'c_q', 'c_k', 'c_v', 'gate_a', 'gate_b', 'gate_c')
IN_SIZES = (CONV_DIM, CONV_DIM, CONV_DIM, HGRN_DIM, HGRN_DIM, HGRN_DIM, HGRN_DIM, HGRN_DIM, ATT_DIM, KV_DIM, KV_DIM, D_MODEL, D_MODEL, D_MODEL)
W_IN_COLS = 3 * CONV_DIM + 5 * HGRN_DIM + ATT_DIM + 2 * KV_DIM + N_BRANCH * D_MODEL

N_EXPERTS = 16
N_GROUPS = 4
EXPERTS_PER_GROUP = N_EXPERTS // N_GROUPS
TOP_K = 2
EXPERT_DFF = 1024
MOE_BLOCK = 128

kernel_name = 'hybrid_conv_hgrn2_gqa_moe_dit'


def rms_norm(x, g):
    xf = x.astype(jnp.float32)
    y = xf * lax.rsqrt(jnp.mean(xf * xf, axis=-1, keepdims=True) + EPS)
    return (y * g.astype(jnp.float32)).astype(x.dtype)


def _project(u, w_in):
    z = u @ w_in
    parts = {}
    off = 0
    for name, size in zip(IN_NAMES, IN_SIZES):
        parts[name] = z[..., off:off + size]
        off += size
    return parts


def _short_conv(u, w):
    L = u.shape[1]
    pad = CONV_WIDTH // 2
    up = jnp.pad(u, ((0, 0), (pad, CONV_WIDTH - 1 - pad), (0, 0)))
    y = up[:, 0:L] * w[0]
    for j in range(1, CONV_WIDTH):
        y = y + up[:, j:j + L] * w[j]
    return y


def _conv_mixer(p, conv_w, w_out):
    return (p['a_b'] * _short_conv(p['a_c'] * p['a_val'], conv_w)) @ w_out


def _hgrn_lower_bounds(lb_param):
    p = jax.nn.softmax(lb_param.astype(jnp.float32), axis=0)
    cum = jnp.cumsum(p, axis=0)
    return cum - cum[0:1]


def _gla_chunk_scan(q, k, v, log_f, s0):
    B, L, H, _ = q.shape
    n = L // HGRN_CHUNK

    def to_chunks(t):
        return jnp.moveaxis(t.reshape(B, n, HGRN_CHUNK, H, t.shape[-1]), 1, 0)

    lower = jnp.tril(jnp.ones((HGRN_CHUNK, HGRN_CHUNK), dtype=bool))[None, :, :, None, None]

    def step(S, inp):
        qc, kc, vc, gc = inp
        b = jnp.cumsum(gc, axis=1)
        o_inter = jnp.einsum('bthk,bhkv->bthv', qc * jnp.exp(b), S)
        diff = b[:, :, None] - b[:, None, :]
        decay = jnp.exp(jnp.where(lower, diff, -jnp.inf))
        scores = jnp.einsum('bthk,bshk,btshk->bhts', qc, kc, decay)
        o_intra = jnp.einsum('bhts,bshv->bthv', scores, vc)
        b_last = b[:, -1]
        S_new = jnp.exp(b_last)[..., None] * S + jnp.einsum(
            'bshk,bshv->bhkv', kc * jnp.exp(b_last[:, None] - b), vc)
        return S_new, o_inter + o_intra

    S_fin, o = lax.scan(step, s0, (to_chunks(q), to_chunks(k), to_chunks(v), to_chunks(log_f)))
    o = jnp.moveaxis(o, 0, 1).reshape(B, L, H, v.shape[-1])
    return S_fin, o


def _hgrn_mixer(p_ctx, p_lat, lb, norm_g):
    B = p_ctx['b_q'].shape[0]

    def heads(t):
        return t.astype(jnp.float32).reshape(t.shape[0], t.shape[1], HGRN_HEADS, HGRN_EXPAND)

    def ident(t):
        return t

    def rev(t):
        return t[:, ::-1]

    q_c, q_l = heads(jax.nn.silu(p_ctx['b_q'])), heads(jax.nn.silu(p_lat['b_q']))
    v_c, v_l = heads(p_ctx['b_i']), heads(p_lat['b_i'])
    s0 = jnp.zeros((B, HGRN_HEADS, HGRN_EXPAND, HGRN_EXPAND), jnp.float32)
    o_ctx = jnp.zeros_like(v_c)
    o_lat = jnp.zeros_like(v_l)
    for direction, (fname, order) in enumerate((('b_f_fwd', ident), ('b_f_bwd', rev))):
        lb_d = lb[direction].reshape(HGRN_HEADS, HGRN_EXPAND)
        f_c = lb_d + (1.0 - lb_d) * jax.nn.sigmoid(heads(p_ctx[fname]))
        f_l = lb_d + (1.0 - lb_d) * jax.nn.sigmoid(heads(p_lat[fname]))
        s_ctx, oc = _gla_chunk_scan(order(q_c), order(1.0 - f_c), order(v_c), order(jnp.log(f_c)), s0)
        _, ol = _gla_chunk_scan(order(q_l), order(1.0 - f_l), order(v_l), order(jnp.log(f_l)), s_ctx)
        o_ctx = o_ctx + order(oc)
        o_lat = o_lat + order(ol)
    g = norm_g.astype(jnp.float32).reshape(HGRN_HEADS, HGRN_EXPAND)

    def readout(o, p):
        o = o * lax.rsqrt(jnp.mean(o * o, axis=-1, keepdims=True) + EPS) * g
        o = o.reshape(o.shape[0], o.shape[1], HGRN_DIM) * jax.nn.silu(p['b_g'].astype(jnp.float32))
        return o.astype(p['b_g'].dtype)

    return readout(o_ctx, p_ctx), readout(o_lat, p_lat)


def _axial_rope(rows):
    row = jnp.repeat(jnp.arange(rows), GRID_W).astype(jnp.float32)
    col = (jnp.arange(rows * GRID_W) % GRID_W).astype(jnp.float32)
    inv = ROPE_THETA ** (-jnp.arange(0, ROPE_AXIS_DIM, 2, dtype=jnp.float32) / ROPE_AXIS_DIM)
    ar = row[:, None] * inv
    ac = col[:, None] * inv
    ang = jnp.concatenate([ar, ar, ac, ac], axis=-1)
    return jnp.cos(ang), jnp.sin(ang)


def _rotate_half(t):
    a, b = jnp.split(t, 2, axis=-1)
    return jnp.concatenate([-b, a], axis=-1)


def _apply_rope(x, cos, sin):
    xf = x.astype(jnp.float32)
    rot = jnp.concatenate([_rotate_half(xf[..., :ROPE_AXIS_DIM]), _rotate_half(xf[..., ROPE_AXIS_DIM:])], axis=-1)
    return (xf * cos[:, None] + rot * sin[:, None]).astype(x.dtype)


def _attn_heads(p, q_g, k_g):
    B, L, _ = p['c_q'].shape
    q = rms_norm(p['c_q'].reshape(B, L, ATT_HEADS, HEAD_DIM), q_g)
    k = rms_norm(p['c_k'].reshape(B, L, ATT_KV_HEADS, HEAD_DIM), k_g)
    v = p['c_v'].reshape(B, L, ATT_KV_HEADS, HEAD_DIM)
    return q, k, v


def _gqa(q, k, v):
    B, Q = q.shape[:2]
    qg = q.reshape(B, Q, ATT_KV_HEADS, ATT_GROUP, HEAD_DIM).astype(jnp.float32)
    s = jnp.einsum('bqkgd,bskd->bkgqs', qg, k.astype(jnp.float32)) * (HEAD_DIM ** -0.5)
    p = jax.nn.softmax(s, axis=-1)
    o = jnp.einsum('bkgqs,bskd->bqkgd', p, v.astype(jnp.float32))
    return o.reshape(B, Q, ATT_DIM).astype(q.dtype)


def _latent_attention(q_l, k_all, v_all):
    B, L = q_l.shape[:2]
    nb = L // Q_BLOCK
    qb = jnp.moveaxis(q_l.reshape(B, nb, Q_BLOCK, ATT_HEADS, HEAD_DIM), 1, 0)
    o = lax.map(lambda blk: _gqa(blk, k_all, v_all), qb)
    return jnp.moveaxis(o, 0, 1).reshape(B, L, ATT_DIM)


def _merge(p, ya, yb, yc, w_o):
    m = (jax.nn.sigmoid(p['gate_a']) * ya + jax.nn.sigmoid(p['gate_b']) * yb
         + jax.nn.sigmoid(p['gate_c']) * yc)
    return m @ w_o


def _route(h, router_w, router_b):
    T = h.shape[0]
    scores = jax.nn.sigmoid((h @ router_w).astype(jnp.float32))
    sel = scores + router_b.astype(jnp.float32)
    grp_score = lax.top_k(sel.reshape(T, N_GROUPS, EXPERTS_PER_GROUP), TOP_K)[0].sum(-1)
    best = jnp.argmax(grp_score, axis=-1)
    in_group = (jnp.arange(N_EXPERTS) // EXPERTS_PER_GROUP)[None, :] == best[:, None]
    _, idx = lax.top_k(jnp.where(in_group, sel, -jnp.inf), TOP_K)
    w = jnp.take_along_axis(scores, idx, axis=-1)
    return idx, w / jnp.sum(w, axis=-1, keepdims=True)


def _moe(h, router_w, router_b, w_gate, w_up, w_down):
    shp = h.shape
    hf = h.reshape(-1, shp[-1])
    T = hf.shape[0]
    idx, gw = _route(hf, router_w, router_b)
    A = T * TOP_K
    flat_e = idx.reshape(-1)
    flat_tok = jnp.repeat(jnp.arange(T, dtype=jnp.int32), TOP_K)
    flat_w = gw.reshape(-1)
    order = jnp.argsort(flat_e)
    se = flat_e[order]
    counts = jnp.bincount(flat_e, length=N_EXPERTS)
    padded = (counts + MOE_BLOCK - 1) // MOE_BLOCK * MOE_BLOCK
    start = jnp.cumsum(counts) - counts
    pend = jnp.cumsum(padded)
    pstart = pend - padded
    dest = pstart[se] + jnp.arange(A) - start[se]
    n_blocks = -(-A // MOE_BLOCK) + N_EXPERTS
    P = n_blocks * MOE_BLOCK
    row_tok = jnp.zeros((P,), jnp.int32).at[dest].set(flat_tok[order])
    row_w = jnp.zeros((P,), hf.dtype).at[dest].set(flat_w[order].astype(hf.dtype))
    blk_e = jnp.minimum(jnp.searchsorted(pend, jnp.arange(n_blocks) * MOE_BLOCK, side='right'), N_EXPERTS - 1)
    xs = hf[row_tok].reshape(n_blocks, MOE_BLOCK, shp[-1])

    def expert_rows(args):
        xb, e = args
        hid = jax.nn.silu(xb @ w_gate[e]) * (xb @ w_up[e])
        return hid @ w_down[e]

    ys = lax.map(expert_rows, (xs, blk_e)).reshape(P, shp[-1])
    out = jnp.zeros_like(hf).at[row_tok].add(ys * row_w[:, None])
    return out.reshape(shp)


def setup_inputs(seed: int = 0) -> dict:
    key = jax.random.key(seed)
    ks = jax.random.split(key, 24)
    f32 = jnp.float32

    def dense(k, shape, fan_in, gain=1.0):
        return jax.random.normal(k, shape, f32) * (gain * fan_in ** -0.5)

    def gain_vec(k, shape):
        return 1.0 + 0.1 * jax.random.normal(k, shape, f32)

    return {
        'x': jax.random.normal(ks[0], (BATCH, SEQ, D_MODEL), f32),
        'c': jax.random.normal(ks[1], (BATCH, D_MODEL), f32),
        'ctx': jax.random.normal(ks[2], (BATCH, CTX_LEN, D_MODEL), f32),
        'c_ctx': jax.random.normal(ks[3], (D_MODEL,), f32),
        'w_mod': dense(ks[4], (DEPTH, D_MODEL, 6 * D_MODEL), D_MODEL, 0.5),
        'b_mod': 0.02 * jax.random.normal(ks[5], (DEPTH, 6 * D_MODEL), f32),
        'norm1_g': gain_vec(ks[6], (DEPTH, D_MODEL)),
        'norm2_g': gain_vec(ks[7], (DEPTH, D_MODEL)),
        'w_in': dense(ks[8], (DEPTH, D_MODEL, W_IN_COLS), D_MODEL),
        'conv_w': dense(ks[9], (DEPTH, CONV_WIDTH, CONV_DIM), CONV_WIDTH),
        'lb_param': jax.random.normal(ks[10], (DEPTH, 2, HGRN_DIM), f32),
        'hgrn_norm_g': gain_vec(ks[11], (DEPTH, HGRN_DIM)),
        'q_norm_g': gain_vec(ks[12], (DEPTH, HEAD_DIM)),
        'k_norm_g': gain_vec(ks[13], (DEPTH, HEAD_DIM)),
        'w_a_out': dense(ks[14], (DEPTH, CONV_DIM, D_MODEL), CONV_DIM),
        'w_b_out': dense(ks[15], (DEPTH, HGRN_DIM, D_MODEL), HGRN_DIM),
        'w_c_out': dense(ks[16], (DEPTH, ATT_DIM, D_MODEL), ATT_DIM),
        'w_o': dense(ks[17], (DEPTH, D_MODEL, D_MODEL), D_MODEL),
        'router_w': dense(ks[18], (D_MODEL, N_EXPERTS), D_MODEL),
        'router_b': 0.01 * jax.random.normal(ks[19], (N_EXPERTS,), f32),
        'w_gate': dense(ks[20], (DEPTH, N_EXPERTS, D_MODEL, EXPERT_DFF), D_MODEL),
        'w_up': dense(ks[21], (DEPTH, N_EXPERTS, D_MODEL, EXPERT_DFF), D_MODEL),
        'w_down': dense(ks[22], (DEPTH, N_EXPERTS, EXPERT_DFF, D_MODEL), EXPERT_DFF),
        'final_g': gain_vec(ks[23], (D_MODEL,)),
    }


def reference(x, c, ctx, c_ctx, w_mod, b_mod, norm1_g, norm2_g, w_in, conv_w, lb_param,
              hgrn_norm_g, q_norm_g, k_norm_g, w_a_out, w_b_out, w_c_out, w_o,
              router_w, router_b, w_gate, w_up, w_down, final_g):
    L = x.shape[1]
    ROWS = L // GRID_W
    cos, sin = _axial_rope(ROWS)
    lower_bounds = _hgrn_lower_bounds(lb_param)
    h_lat, h_ctx = x, ctx
    for layer in range(DEPTH):
        last = layer == DEPTH - 1
        mod_lat = jnp.split((jax.nn.silu(c) @ w_mod[layer] + b_mod[layer])[:, None, :], 6, axis=-1)
        mod_ctx = jnp.split((jax.nn.silu(c_ctx) @ w_mod[layer] + b_mod[layer])[None, None, :], 6, axis=-1)
        sh1_l, sc1_l, g1_l, sh2_l, sc2_l, g2_l = mod_lat
        sh1_c, sc1_c, g1_c, sh2_c, sc2_c, g2_c = mod_ctx

        u_lat = rms_norm(h_lat, norm1_g[layer]) * (1.0 + sc1_l) + sh1_l
        u_ctx = rms_norm(h_ctx, norm1_g[layer]) * (1.0 + sc1_c) + sh1_c
        p_lat = _project(u_lat, w_in[layer])
        p_ctx = _project(u_ctx, w_in[layer])

        yb_ctx, yb_lat = _hgrn_mixer(p_ctx, p_lat, lower_bounds[layer], hgrn_norm_g[layer])

        q_c, k_c, v_c = _attn_heads(p_ctx, q_norm_g[layer], k_norm_g[layer])
        q_l, k_l, v_l = _attn_heads(p_lat, q_norm_g[layer], k_norm_g[layer])
        q_l = _apply_rope(q_l, cos, sin)
        k_l = _apply_rope(k_l, cos, sin)
        k_all = jnp.concatenate([k_c, k_l], axis=1)
        v_all = jnp.concatenate([v_c, v_l], axis=1)
        yc_lat = _latent_attention(q_l, k_all, v_all)

        ya_lat = _conv_mixer(p_lat, conv_w[layer], w_a_out[layer])
        out_lat = _merge(p_lat, ya_lat, yb_lat @ w_b_out[layer], yc_lat @ w_c_out[layer], w_o[layer])
        h_lat = h_lat + g1_l * out_lat
        if not last:
            ya_ctx = _conv_mixer(p_ctx, conv_w[layer], w_a_out[layer])
            yc_ctx = _gqa(q_c, k_c, v_c)
            out_ctx = _merge(p_ctx, ya_ctx, yb_ctx @ w_b_out[layer], yc_ctx @ w_c_out[layer], w_o[layer])
            h_ctx = h_ctx + g1_c * out_ctx

        m_lat = rms_norm(h_lat, norm2_g[layer]) * (1.0 + sc2_l) + sh2_l
        h_lat = h_lat + g2_l * _moe(m_lat, router_w, router_b, w_gate[layer], w_up[layer], w_down[layer])
        if not last:
            m_ctx = rms_norm(h_ctx, norm2_g[layer]) * (1.0 + sc2_c) + sh2_c
            h_ctx = h_ctx + g2_c * _moe(m_ctx, router_w, router_b, w_gate[layer], w_up[layer], w_down[layer])
    return rms_norm(h_lat, final_g)
```

```python
import numpy as np
import concourse.bass as bass
import concourse.mybir as mybir
from concourse.bass_utils import run_bass_kernel_spmd

F32 = mybir.dt.float32
BF16 = mybir.dt.bfloat16
AF = mybir.ActivationFunctionType
ALU = mybir.AluOpType
AX = mybir.AxisListType


class Reg:
    __slots__ = ("name", "w", "rs")

    def __init__(self, name):
        self.name = name
        self.w = None
        self.rs = []


class _Op:
    __slots__ = ("eng", "fn", "deps", "idx", "signal", "dma", "barrier")


class Sched:
    ENGS = ("pe", "act", "dve", "pool", "sp")

    def __init__(self, nc, n_dma_sems=24):
        self.nc = nc
        self.h = {"pe": nc.tensor, "act": nc.scalar, "dve": nc.vector, "pool": nc.gpsimd, "sp": nc.sync}
        self.ops = {e: [] for e in self.ENGS}
        self.n_dma_sems = n_dma_sems
        self.dma_val = [0] * n_dma_sems
        self.dma_rr = 0
        self.n_sw = 8
        self.rr_hw = 0
        self.rr_sw = 0
        self.out_tokens = []

    def _collect(self, reads, writes):
        deps = []
        for r in reads:
            if r.w is not None:
                deps.append(r.w)
        for w in writes:
            if w.w is not None:
                deps.append(w.w)
            deps.extend(w.rs)
        return deps

    def _commit(self, tok, reads, writes):
        for r in reads:
            r.rs.append(tok)
            if len(r.rs) > 64:
                last = {}
                for t in r.rs:
                    last[(t[0], t[1])] = t if (t[0], t[1]) not in last or t[2] > last[(t[0], t[1])][2] else last[(t[0], t[1])]
                r.rs = list(last.values())
        for w in writes:
            w.w = tok
            w.rs = []

    def op(self, eng, fn, reads=(), writes=()):
        o = _Op()
        o.eng = eng
        o.fn = fn
        o.deps = self._collect(reads, writes)
        o.idx = len(self.ops[eng])
        o.signal = False
        o.dma = None
        o.barrier = False
        self.ops[eng].append(o)
        self._commit(("E", eng, o.idx), reads, writes)
        return o

    def dma(self, eng, out, in_, reads=(), writes=(), is_output=False, **kw):
        o = _Op()
        o.eng = eng
        if eng == "pool":
            k = self.n_dma_sems - self.n_sw + self.rr_sw
            self.rr_sw = (self.rr_sw + 1) % self.n_sw
        else:
            k = self.rr_hw
            self.rr_hw = (self.rr_hw + 1) % (self.n_dma_sems - self.n_sw)
        o.deps = self._collect(reads, writes)
        if self.dma_val[k] > 0:
            o.deps.append(("D", k, self.dma_val[k]))
        self.dma_val[k] += 16
        o.dma = (k, self.dma_val[k])
        o.fn = lambda h: h.dma_start(out=out, in_=in_, **kw)
        o.idx = len(self.ops[eng])
        o.signal = False
        o.barrier = False
        self.ops[eng].append(o)
        tok = ("D", k, self.dma_val[k])
        self._commit(tok, reads, writes)
        if is_output:
            self.out_tokens.append(tok)
        return o

    def cc(self, fn, reads=(), writes=()):
        o = _Op()
        o.eng = "pool"
        k = self.n_dma_sems - self.n_sw + self.rr_sw
        self.rr_sw = (self.rr_sw + 1) % self.n_sw
        o.deps = self._collect(reads, writes)
        if self.dma_val[k] > 0:
            o.deps.append(("D", k, self.dma_val[k]))
        self.dma_val[k] += 16
        o.dma = (k, self.dma_val[k])
        o.fn = fn
        o.idx = len(self.ops["pool"])
        o.signal = False
        o.barrier = False
        self.ops["pool"].append(o)
        self._commit(("D", k, self.dma_val[k]), reads, writes)
        return o

    def barrier(self):
        toks = []
        for e in self.ENGS:
            for i in range(len(self.ops[e]) - 1, -1, -1):
                o = self.ops[e][i]
                if o.fn is not None and o.dma is None:
                    toks.append(("E", e, i))
                    break
        for k in range(self.n_dma_sems):
            if self.dma_val[k] > 0:
                toks.append(("D", k, self.dma_val[k]))
        for e in self.ENGS:
            o = _Op()
            o.eng = e
            o.fn = None
            o.deps = [t for t in toks if not (t[0] == "E" and t[1] == e)]
            o.idx = len(self.ops[e])
            o.signal = False
            o.dma = None
            o.barrier = True
            self.ops[e].append(o)

    def emit(self):
        nc = self.nc
        self.barrier()
        for e in self.ENGS:
            for o in self.ops[e]:
                for t in o.deps:
                    if t[0] == "E":
                        if t[1] == e and e == "pe":
                            continue
                        self.ops[t[1]][t[2]].signal = True
        for e in self.ENGS:
            lst = self.ops[e]
            for i, o in enumerate(lst):
                if (o.fn is None or o.dma is not None) and o.signal:
                    raise RuntimeError("barrier/dma op marked as signal")
        cnt = {}
        for e in self.ENGS:
            c = 0
            arr = []
            for o in self.ops[e]:
                if o.signal:
                    c += 1
                arr.append(c)
            cnt[e] = arr
        import contextlib
        with contextlib.ExitStack() as st:
            esem = {e: st.enter_context(nc.semaphore("s_" + e)) for e in self.ENGS}
            dsem = [st.enter_context(nc.semaphore("d_%d" % k)) for k in range(self.n_dma_sems)]
            block = st.enter_context(nc.Block())

            def emit_engine(e, h):
                waited = {}
                for o in self.ops[e]:
                    need = {}
                    for t in o.deps:
                        if t[0] == "E":
                            if t[1] == e and e == "pe":
                                continue
                            key = ("E", t[1])
                            v = cnt[t[1]][t[2]]
                        else:
                            key = ("D", t[1])
                            v = t[2]
                        if v > need.get(key, 0):
                            need[key] = v
                    for key, v in need.items():
                        if waited.get(key, 0) >= v:
                            continue
                        waited[key] = v
                        sem = esem[key[1]] if key[0] == "E" else dsem[key[1]]
                        h.wait_ge(sem, v)
                    if o.fn is None:
                        continue
                    inst = o.fn(h)
                    if o.dma is not None:
                        inst.then_inc(dsem[o.dma[0]], 16)
                    elif o.signal:
                        inst.then_inc(esem[e], 1)

            @block.tensor
            def _(h):
                emit_engine("pe", h)

            @block.scalar
            def _(h):
                emit_engine("act", h)

            @block.vector
            def _(h):
                emit_engine("dve", h)

            @block.gpsimd
            def _(h):
                emit_engine("pool", h)

            @block.sync
            def _(h):
                emit_engine("sp", h)
        return nc


import contextlib

D = 1024
CT = 256
EPS = 1e-6
O_AVAL, O_AB, O_AC = 0, 512, 1024
O_BQ, O_BFF, O_BFB, O_BI, O_BG = 1536, 2048, 2560, 3072, 3584
O_CQ, O_CK, O_CV = 4096, 4608, 4736
O_GA = 4864
WCOLS = 7936


def build(T, mode="F", layer=None, stop=None, dbg=None):
    nc = bass.Bass("TRN2", target_bir_lowering=False)
    S = Sched(nc)
    layers = (0, 1) if mode == "F" else (layer,)
    NLW = 2 if mode == "F" else 1
    wl = (lambda l: l) if mode == "F" else (lambda l: 0)
    need_moe = (mode != "A") and (stop in (None, "moe"))
    in_names = []
    TT = T + CT
    NCH = TT // 128

    def din(name, shape, dt=F32):
        in_names.append(name)
        return nc.dram_tensor(name, list(shape), dt, kind="ExternalInput").ap()

    def dout(name, shape, dt=F32):
        return nc.dram_tensor(name, list(shape), dt, kind="ExternalOutput").ap()

    def dscr(name, shape, dt):
        return nc.dram_tensor(name, list(shape), dt, kind="Internal").ap()

    x = din("x", [T, D])
    ctx_in = din("ctx", [CT, D])
    cc = din("cc", [128, 8, 2])
    w_mod = din("w_mod", [NLW, D, 6 * D])
    b_modT = din("b_modT", [NLW, 128, 48])
    n1g = din("n1g", [NLW, 128, 8])
    n2g = din("n2g", [NLW, 128, 8])
    w_in = din("w_in", [NLW, D, WCOLS])
    conv_w = din("conv_w", [NLW, 128, 4, 3])
    lbp = din("lbp", [128, 2, 2, 4])
    hng = din("hng", [NLW, 128, 4])
    qng = din("qng", [NLW, 64, 1])
    kng = din("kng", [NLW, 64, 1])
    w_a = din("w_a_out", [NLW, 512, D])
    w_b = din("w_b_out", [NLW, 512, D])
    w_c = din("w_c_out", [NLW, 512, D])
    w_o = din("w_o", [NLW, D, D])
    rw = din("router_w", [D, 16])
    rb = din("rb", [128, 16])
    if need_moe:
        w_gate = din("w_gate", [NLW, 16, D, D])
        w_up = din("w_up", [NLW, 16, D, D])
        w_down = din("w_down", [NLW, 16, D, D])
    fgc = din("fgc", [128, 8])
    msk = din("msk", [128, 16])
    cosT = din("cosT", [64, T])
    sinT = din("sinT", [64, T])
    cst = din("cst", [128, 8, 128])
    has_out = (mode == "F") or (mode == "B" and layer == 1)
    out = dout("out", [T, D]) if has_out else None
    if mode == "B" and layer == 0:
        HL = dout("HL", [T, D])
        HC = dout("HC", [CT, D])
    else:
        HL = dscr("HL", [T, D], F32)
        HC = dscr("HC", [CT, D], F32)
    H1 = dscr("H1", [TT, D], F32)
    UT = dscr("UT", [D, TT], BF16)
    if mode == "A":
        KX = dout("KX", [128, T + 16], BF16)
        VX = dout("VX", [T, 130], BF16)
        SX = dout("SX", [128, 1032], F32)
    else:
        KX = dscr("KX", [128, T + 16], BF16)
        VX = dscr("VX", [T, 130], BF16)
        SX = dscr("SX", [128, 1032], F32)
    if mode == "B":
        KG = din("KG", [512, T + 16], BF16)
        VG = din("VG", [4 * T, 130], BF16)
        SG = din("SG", [512, 1032], F32)
    else:
        KG = dscr("KG", [512, T + 16], BF16)
        VG = dscr("VG", [4 * T, 130], BF16)
        SG = dscr("SG", [512, 1032], F32)
    KC = dscr("KC", [128, CT], BF16)
    VC = dscr("VC", [CT, 130], BF16)
    YB = dscr("YB", [512, TT], BF16)
    PA = dscr("PA", [512, TT], BF16)
    YC = dscr("YC", [512, TT], BF16)
    OF = dscr("OF", [TT, 512], F32)
    if False:
        WGb = dscr("WGb", [2, 16, D, D], BF16)
    WIb = dscr("WIb", [2, D, WCOLS], BF16)
    dbg_outs = []
    if dbg is not None:
        for (dn, dshape, ddt) in dbg:
            dbg_outs.append((dn, nc.dram_tensor("dbg_" + dn, list(dshape), ddt, kind="ExternalOutput").ap()))

    R = {}

    def rg(n):
        if n not in R:
            R[n] = Reg(n)
        return R[n]

    RG = rg
    GROUPS = [[0, 1, 2, 3], [4, 5, 6, 7]]

    with contextlib.ExitStack() as top:
        _sbn = [0]

        def SB(st, name, shape, dt):
            _sbn[0] += 1
            return st.enter_context(nc.sbuf_tensor("%s_%d" % (name, _sbn[0]), list(shape), dt))

        C = SB(top, "cst", [128, 8, 128], F32)
        Cb = SB(top, "cstb", [128, 2, 128], BF16)
        epsb = SB(top, "epsb", [128, 1], F32)
        mskt = SB(top, "mskt", [128, 16], F32)
        modc = SB(top, "modc", [128, 48, 2], F32)
        a1 = SB(top, "a1", [128, 8, 2], F32)
        a2 = SB(top, "a2", [128, 8, 2], F32)
        g1b = SB(top, "g1b", [128, 2, D], F32)
        g2b = SB(top, "g2b", [128, 2, D], F32)
        fgb = SB(top, "fgb", [128, D], F32)
        lbb = SB(top, "lbb", [128, 2, 512], F32)
        omb = SB(top, "omb", [128, 2, 512], F32)
        hgb = SB(top, "hgb", [128, 512], F32)
        rbt = SB(top, "rbt", [128, 16], F32)
        sctx = SB(top, "sctx", [128, 2, 512], F32)
        scin = SB(top, "scin", [128, 2, 512], F32)
        ccs = SB(top, "ccs", [128, 8, 2], F32)
        PS = [top.enter_context(nc.psum_tensor("ps%d" % i, [128, 512], F32)) for i in range(8)]
        PSR = [rg("ps%d" % i) for i in range(8)]
        ident = C[:, 0, :]
        ones = C[:, 5, :]

        S.dma("sp", C[:], cst, writes=[rg("C")])
        S.dma("sp", mskt[:], msk, writes=[rg("mskt")])
        S.dma("sp", rbt[:], rb, writes=[rg("rbt")])
        S.dma("sp", ccs[:], cc, writes=[rg("ccs")])
        S.op("pool", lambda h: h.memset(epsb[:], EPS), writes=[rg("epsb")])
        S.op("dve", lambda h: h.tensor_copy(out=Cb[:, 0, :], in_=C[:, 0, :]), reads=[rg("C")], writes=[rg("Cb")])
        S.op("act", lambda h: h.activation(out=ccs[:], in_=ccs[:], func=AF.Silu), reads=[rg("ccs")], writes=[rg("ccs")])
        identb = Cb[:, 0, :]

        for l in layers:
            for rr in range(8):
                S.dma("pool", WIb[l, rr * 128:(rr + 1) * 128, :], w_in[wl(l), rr * 128:(rr + 1) * 128, :], writes=[rg("WIb%d" % l)])
        for l in ():
            for e in range(16):
                for rr in range(8):
                    rs_ = slice(rr * 128, (rr + 1) * 128)
                    S.dma("pool", WGb[l, e, rs_, :], w_gate[wl(l), e, rs_, :], writes=[rg("WE%d" % l)])
                    S.dma("pool", WUb[l, e, rs_, :], w_up[wl(l), e, rs_, :], writes=[rg("WE%d" % l)])
                    S.dma("pool", WDb[l, e, rs_, :], w_down[wl(l), e, rs_, :], writes=[rg("WE%d" % l)])

        psn = [0]

        def ps():
            i = psn[0] % 8
            psn[0] += 1
            return PS[i], PSR[i]

        def bcast(st, col, nchunk, dst, dstreg, colreg):
            dg = SB(st, "dg%d" % psn[0], [128, 128], F32)
            for c in range(nchunk):
                S.op("dve", lambda h, c=c: h.tensor_scalar(out=dg[:], in0=ident, scalar1=col[:, c:c + 1], scalar2=None, op0=ALU.mult),
                     reads=[rg("C"), colreg], writes=[rg("dg")])
                p, pr = ps()
                S.op("pe", lambda h, p=p: h.matmul(p[:, 0:128], lhsT=ones, rhs=dg[:], start=True, stop=True),
                     reads=[rg("C"), rg("dg")], writes=[pr])
                S.op("act", lambda h, p=p, c=c: h.activation(out=dst[:, c * 128:(c + 1) * 128], in_=p[:, 0:128], func=AF.Copy),
                     reads=[pr], writes=[dstreg])

        def rstd_from_ss(ss, n, scale, reg):
            S.op("act", lambda h: h.activation(out=ss, in_=ss, func=AF.Ln, bias=epsb[:, 0:1], scale=scale), reads=[reg, rg("epsb")], writes=[reg])
            S.op("act", lambda h: h.activation(out=ss, in_=ss, func=AF.Exp, scale=-0.5), reads=[reg], writes=[reg])

        with contextlib.ExitStack() as st:
            fgc_t = SB(st, "fgc_t", [128, 8], F32)
            S.dma("sp", fgc_t[:], fgc, writes=[rg("fgc_t")])
            bcast(st, fgc_t, 8, fgb, rg("fgb"), rg("fgc_t"))
            S.barrier()

        def layer_setup(l):
            with contextlib.ExitStack() as st:
                wm = [SB(st, "wm%d" % i, [128, 8, 512], F32) for i in range(2)]
                bmt = SB(st, "bmt", [128, 48], F32)
                ng1 = SB(st, "ng1", [128, 8], F32)
                ng2 = SB(st, "ng2", [128, 8], F32)
                lbt = SB(st, "lbt", [128, 2, 2, 4], F32)
                hgt = SB(st, "hgt", [128, 4], F32)
                tmp = SB(st, "tmpm", [128, 8, 2], F32)
                S.dma("sp", bmt[:], b_modT[wl(l)], writes=[rg("bmt")])
                S.dma("sp", ng1[:], n1g[wl(l)], writes=[rg("ng1")])
                S.dma("sp", ng2[:], n2g[wl(l)], writes=[rg("ng2")])
                S.dma("sp", lbt[:], lbp, writes=[rg("lbt")])
                S.dma("sp", hgt[:], hng[wl(l)], writes=[rg("hgt")])
                pm, pmr = ps()
                for piece in range(12):
                    wt = wm[piece % 2]
                    wr = rg("wm%d" % (piece % 2))
                    S.dma("sp", wt[:], w_mod[wl(l)].rearrange("(kc p) n -> p kc n", p=128)[:, :, piece * 512:(piece + 1) * 512], writes=[wr])
                    for jj in range(4):
                        j = piece * 4 + jj
                        for kc in range(8):
                            S.op("pe", lambda h, wt=wt, jj=jj, j=j, kc=kc: h.matmul(pm[:, 2 * j:2 * j + 2], lhsT=wt[:, kc, jj * 128:(jj + 1) * 128],
                                                                                     rhs=ccs[:, kc, :], start=(kc == 0), stop=(kc == 7)),
                                 reads=[wr, rg("ccs")], writes=[pmr])
                for w in range(2):
                    S.op("dve", lambda h, w=w: h.tensor_tensor(out=modc[:, :, w], in0=pm[:, 0:96].rearrange("p (j w) -> p j w", w=2)[:, :, w], in1=bmt[:], op=ALU.add),
                         reads=[pmr, rg("bmt")], writes=[rg("modc")])
                for w in range(2):
                    S.op("dve", lambda h, w=w: h.scalar_tensor_tensor(out=a1[:, :, w], in0=modc[:, 8:16, w], scalar=1.0, in1=ng1[:], op0=ALU.add, op1=ALU.mult),
                         reads=[rg("modc"), rg("ng1")], writes=[rg("a1")])
                    S.op("dve", lambda h, w=w: h.scalar_tensor_tensor(out=a2[:, :, w], in0=modc[:, 32:40, w], scalar=1.0, in1=ng2[:], op0=ALU.add, op1=ALU.mult),
                         reads=[rg("modc"), rg("ng2")], writes=[rg("a2")])
                for w in range(2):
                    S.op("dve", lambda h, w=w: h.tensor_copy(out=tmp[:, :, w], in_=modc[:, 16:24, w]), reads=[rg("modc")], writes=[rg("tmpm")])
                    bcast(st, tmp[:, :, w], 8, g1b[:, w, :], rg("g1b"), rg("tmpm"))
                for w in range(2):
                    S.op("dve", lambda h, w=w: h.tensor_copy(out=tmp[:, :, w], in_=modc[:, 40:48, w]), reads=[rg("modc")], writes=[rg("tmpm")])
                    bcast(st, tmp[:, :, w], 8, g2b[:, w, :], rg("g2b"), rg("tmpm"))
                lbc = SB(st, "lbc", [128, 2, 4], F32)
                if l == 0:
                    S.op("dve", lambda h: h.memset(lbc[:], 0.0), writes=[rg("lbc")])
                else:
                    S.op("dve", lambda h: h.tensor_tensor(out=lbc[:], in0=lbt[:, 1, :, :], in1=lbt[:, 0, :, :], op=ALU.subtract),
                         reads=[rg("lbt")], writes=[rg("lbc")])
                    S.op("act", lambda h: h.activation(out=lbc[:], in_=lbc[:], func=AF.Sigmoid), reads=[rg("lbc")], writes=[rg("lbc")])
                for d in range(2):
                    bcast(st, lbc[:, d, :], 4, lbb[:, d, :], rg("lbb"), rg("lbc"))
                S.op("dve", lambda h: h.tensor_scalar(out=omb[:], in0=lbb[:], scalar1=-1.0, scalar2=1.0, op0=ALU.mult, op1=ALU.add),
                     reads=[rg("lbb")], writes=[rg("omb")])
                bcast(st, hgt, 4, hgb, rg("hgb"), rg("hgt"))
                S.barrier()

        def norm_chunk(st_bufs, src_rows, a_t, sh_lo, which, want_f32):
            ht, junk, ss, hn, ub, uf = st_bufs
            S.dma("sp", ht[:], src_rows, writes=[rg("ht")])
            S.op("dve", lambda h: h.memset(ss[:, 0:1], 0.0), writes=[rg("ss")])
            S.op("act", lambda h: h.activation(out=junk[:], in_=ht[:], func=AF.Square, accum_out=ss[:, 0:1]), reads=[rg("ht"), rg("ss")], writes=[rg("junk"), rg("ss")])
            rstd_from_ss(ss[:, 0:1], 1, 1.0 / D, rg("ss"))
            S.op("dve", lambda h: h.tensor_scalar(out=hn[:], in0=ht[:], scalar1=ss[:, 0:1], scalar2=None, op0=ALU.mult), reads=[rg("ht"), rg("ss")], writes=[rg("hn")])
            for half in range(2):
                p, pr = ps()
                for q in range(4):
                    kc = half * 4 + q
                    S.op("pe", lambda h, p=p, q=q, kc=kc: h.transpose(p[:, q * 128:(q + 1) * 128], hn[:, kc * 128:(kc + 1) * 128], ident),
                         reads=[rg("hn"), rg("C")], writes=[pr])
                for q in range(4):
                    kc = half * 4 + q
                    eng = "act" if q % 2 == 0 else "dve"
                    if eng == "act":
                        S.op("act", lambda h, p=p, q=q, kc=kc: h.activation(out=ub[:, kc, :], in_=p[:, q * 128:(q + 1) * 128], func=AF.Identity,
                                                                         scale=a_t[:, kc, which:which + 1], bias=modc[:, sh_lo + kc, which:which + 1]),
                             reads=[pr, rg("a1"), rg("a2"), rg("modc")], writes=[rg("ub")])
                    else:
                        S.op("dve", lambda h, p=p, q=q, kc=kc: h.tensor_scalar(out=ub[:, kc, :], in0=p[:, q * 128:(q + 1) * 128], scalar1=a_t[:, kc, which:which + 1],
                                                                            scalar2=modc[:, sh_lo + kc, which:which + 1], op0=ALU.mult, op1=ALU.add),
                             reads=[pr, rg("a1"), rg("a2"), rg("modc")], writes=[rg("ub")])
                    if want_f32:
                        S.op("dve", lambda h, p=p, q=q, kc=kc: h.tensor_scalar(out=uf[:, kc, :], in0=p[:, q * 128:(q + 1) * 128], scalar1=a_t[:, kc, which:which + 1],
                                                                            scalar2=modc[:, sh_lo + kc, which:which + 1], op0=ALU.mult, op1=ALU.add),
                             reads=[pr, rg("a1"), rg("a2"), rg("modc")], writes=[rg("uf")])

        def hsrc(l, c):
            if c < 2:
                src = ctx_in if (l == 0 or mode != "F") else HC
                return src[c * 128:(c + 1) * 128, :]
            src = x if (l == 0 or mode != "F") else HL
            return src[(c - 2) * 128:(c - 1) * 128, :]

        UTv = UT.rearrange("(kc p) t -> p kc t", p=128)

        def sweep_n1(l):
            with contextlib.ExitStack() as st:
                ht = SB(st, "ht", [128, D], F32)
                junk = SB(st, "junk", [128, D], BF16)
                ss = SB(st, "ss", [128, 4], F32)
                hn = SB(st, "hn", [128, D], F32)
                ub = SB(st, "ub", [128, 8, 128], BF16)
                for c in range(NCH):
                    norm_chunk((ht, junk, ss, hn, ub, None), hsrc(l, c), a1, 0, 1 if c < 2 else 0, False)
                    S.dma("sp", UTv[:, :, c * 128:(c + 1) * 128], ub[:], reads=[rg("ub")], writes=[rg("UT")])
                S.barrier()

        def qk_norm_rope(st_bufs, src, srcreg, n, gcol, greg, cos_t, sin_t, dst, dstreg):
            xq, sq, rs, xn, t1 = st_bufs
            S.op("act", lambda h: h.activation(out=xq[:, 0:n], in_=src, func=AF.Copy), reads=[srcreg], writes=[rg("xq")])
            S.op("act", lambda h: h.activation(out=sq[:, 0:n], in_=src, func=AF.Square), reads=[srcreg], writes=[rg("sq")])
            p, pr = ps()
            S.op("pe", lambda h: h.matmul(p[0:64, 0:n], lhsT=C[0:64, 5, 0:64], rhs=sq[:, 0:n], start=True, stop=True), reads=[rg("C"), rg("sq")], writes=[pr])
            S.op("act", lambda h: h.activation(out=rs[:, 0:n], in_=p[0:64, 0:n], func=AF.Ln, bias=epsb[0:64, 0:1], scale=1.0 / 64), reads=[pr, rg("epsb")], writes=[rg("rs")])
            S.op("act", lambda h: h.activation(out=rs[:, 0:n], in_=rs[:, 0:n], func=AF.Exp, scale=-0.5), reads=[rg("rs")], writes=[rg("rs")])
            S.op("dve", lambda h: h.scalar_tensor_tensor(out=xn[:, 0:n], in0=xq[:, 0:n], scalar=gcol, in1=rs[:, 0:n], op0=ALU.mult, op1=ALU.mult),
                 reads=[rg("xq"), rg("rs"), greg], writes=[rg("xn")])
            if cos_t is None:
                S.op("dve", lambda h: h.tensor_copy(out=dst, in_=xn[:, 0:n]), reads=[rg("xn")], writes=[dstreg])
                return
            p2, pr2 = ps()
            S.op("pe", lambda h: h.matmul(p2[0:64, 0:n], lhsT=C[0:64, 6, 0:64], rhs=xn[:, 0:n], start=True, stop=True), reads=[rg("C"), rg("xn")], writes=[pr2])
            S.op("dve", lambda h: h.tensor_tensor(out=t1[:, 0:n], in0=p2[0:64, 0:n], in1=sin_t, op=ALU.mult), reads=[pr2, rg("rope")], writes=[rg("t1")])
            S.op("pool", lambda h: h.tensor_tensor(out=xn[:, 0:n], in0=xn[:, 0:n], in1=cos_t, op=ALU.mult), reads=[rg("xn"), rg("rope")], writes=[rg("xn")])
            S.op("dve", lambda h: h.tensor_tensor(out=dst, in0=xn[:, 0:n], in1=t1[:, 0:n], op=ALU.add), reads=[rg("xn"), rg("t1")], writes=[dstreg])

        WIv = [WIb[l].rearrange("(kc p) n -> p kc n", p=128) for l in range(2)]

        def sweep_kv(l):
            with contextlib.ExitStack() as st:
                wk = SB(st, "wk", [128, 8, 128], BF16)
                wv = SB(st, "wv", [128, 8, 128], BF16)
                gk = SB(st, "gk", [64, 1], F32)
                ut = SB(st, "ut", [128, 8, 256], BF16)
                bufs = tuple(SB(st, n, [64, 512], F32) for n in ("xq", "sq", "rs", "xn", "t1"))
                cs = SB(st, "cs", [64, 2, 256], F32)
                ko = SB(st, "ko", [64, 2, 256], BF16)
                vo = SB(st, "vo", [128, 2, 2, 65], BF16)
                ux = SB(st, "ux", [128, 16], BF16)
                S.dma("sp", wk[:], WIv[l][:, :, O_CK:O_CK + 128], reads=[rg("WIb%d" % l)], writes=[rg("wk")])
                S.dma("sp", wv[:], WIv[l][:, :, O_CV:O_CV + 128], reads=[rg("WIb%d" % l)], writes=[rg("wv")])
                S.dma("sp", gk[:], kng[wl(l)], writes=[rg("gk")])
                S.op("pool", lambda h: h.memset(vo[:], 1.0), writes=[rg("vo")])
                for tl in range(TT // 256):
                    t0 = tl * 256
                    isctx = tl == 0
                    S.dma("sp", ut[:], UTv[:, :, t0:t0 + 256], reads=[rg("UT")], writes=[rg("ut")])
                    if not isctx:
                        S.dma("sp", cs[:, 0, :], cosT[:, t0 - CT:t0 - CT + 256], writes=[rg("rope")])
                        S.dma("sp", cs[:, 1, :], sinT[:, t0 - CT:t0 - CT + 256], writes=[rg("rope")])
                    for kvh in range(2):
                        p, pr = ps()
                        for kc in range(8):
                            S.op("pe", lambda h, p=p, kc=kc, kvh=kvh: h.matmul(p[0:64, 0:256], lhsT=wk[:, kc, kvh * 64:(kvh + 1) * 64], rhs=ut[:, kc, :],
                                                                             start=(kc == 0), stop=(kc == 7)), reads=[rg("wk"), rg("ut")], writes=[pr])
                        qk_norm_rope(bufs, p[0:64, 0:256], pr, 256, gk[:, 0:1], rg("gk"), None if isctx else cs[:, 0, :], None if isctx else cs[:, 1, :],
                                     ko[:, kvh, :], rg("ko"))
                    for kvh in range(2):
                        if isctx:
                            S.dma("sp", KC[kvh * 64:(kvh + 1) * 64, :], ko[:, kvh, :], reads=[rg("ko")], writes=[rg("KC")])
                        else:
                            S.dma("sp", KX[kvh * 64:(kvh + 1) * 64, t0 - CT:t0 - CT + 256], ko[:, kvh, :], reads=[rg("ko")], writes=[rg("KX")])
                    for sub in range(2):
                        p, pr = ps()
                        for kc in range(8):
                            S.op("pe", lambda h, p=p, kc=kc, sub=sub: h.matmul(p[:, 0:128], lhsT=ut[:, kc, sub * 128:(sub + 1) * 128], rhs=wv[:, kc, :],
                                                                             start=(kc == 0), stop=(kc == 7)), reads=[rg("wv"), rg("ut")], writes=[pr])
                        S.op("act", lambda h, p=p, sub=sub: h.activation(out=vo[:, sub, :, 0:64], in_=p[:, 0:128].rearrange("p (a d) -> p a d", a=2), func=AF.Copy),
                             reads=[pr], writes=[rg("vo")])
                    dst = VC if isctx else VX
                    r0 = t0 if isctx else t0 - CT
                    S.dma("sp", dst[r0:r0 + 256, :].rearrange("(s p) c -> p s c", p=128), vo[:].rearrange("p s a c -> p s (a c)"),
                          reads=[rg("vo")], writes=[rg("VC" if isctx else "VX")])
                S.dma("sp", ux[:, 0:8].rearrange("p (k o) -> p k o", o=1), UTv[:, :, CT:CT + 1], reads=[rg("UT")], writes=[rg("ux")], allow_slow_non_contiguous=True)
                S.dma("sp", ux[:, 8:16].rearrange("p (k o) -> p k o", o=1), UTv[:, :, TT - 1:TT], reads=[rg("UT")], writes=[rg("ux")], allow_slow_non_contiguous=True)
                S.dma("sp", KX[:, T:T + 16], ux[:], reads=[rg("ux")], writes=[rg("KX")])
                S.barrier()

        def sweep_hgrn(l, full):
            with contextlib.ExitStack() as st:
                wq = SB(st, "hwq", [128, 8, 512], BF16)
                wf = SB(st, "hwf", [128, 2, 8, 512], BF16)
                wi = SB(st, "hwi", [128, 8, 512], BF16)
                wg = SB(st, "hwg", [128, 8, 512], BF16)
                ut = SB(st, "hut", [128, 8, 128], BF16)
                f_t = SB(st, "f_t", [128, 512], F32)
                g_t = SB(st, "g_t", [128, 512], F32)
                k_t = SB(st, "k_t", [128, 512], F32)
                q_t = SB(st, "q_t", [128, 512], F32)
                eP = SB(st, "eP", [128, 512], F32)
                eM = SB(st, "eM", [128, 512], F32)
                qb = SB(st, "qb", [128, 512], F32)
                kb = SB(st, "kb", [128, 512], F32)
                vb = SB(st, "vb", [128, 512], BF16)
                qT = SB(st, "qT", [128, 512], BF16)
                kT = SB(st, "kT", [128, 512], BF16)
                scT = SB(st, "scT", [128, 512], BF16)
                el = SB(st, "el", [128, 4, 5], F32)
                kb4 = SB(st, "kb4", [128, 4, 512], BF16)
                qT4 = SB(st, "qT4", [128, 4, 512], BF16)
                Tb4 = SB(st, "Tb4", [128, 4, 512], BF16)
                Ss = SB(st, "Ss", [128, 512], F32)
                Tf = SB(st, "Tf", [128, 512], F32)
                Tb = SB(st, "Tb", [128, 512], BF16)
                Dl = SB(st, "Dl", [128, 4], F32)
                o_t = SB(st, "o_t", [128, 512], F32)
                of_t = SB(st, "of_t", [128, 512], F32)
                zg = SB(st, "zg", [128, 512], F32)
                oss = SB(st, "oss", [128, 4], F32)
                yb = SB(st, "yb", [128, 512], F32)
                ybT = SB(st, "ybT", [128, 4, 128], BF16)
                junk = SB(st, "hjunk", [128, 128], BF16)
                WR = rg("WIb%d" % l)
                S.dma("sp", wq[:], WIv[l][:, :, O_BQ:O_BQ + 512], reads=[WR], writes=[rg("hwq")])
                S.dma("sp", wf[:, 0], WIv[l][:, :, O_BFF:O_BFF + 512], reads=[WR], writes=[rg("hwf")])
                S.dma("sp", wf[:, 1], WIv[l][:, :, O_BFB:O_BFB + 512], reads=[WR], writes=[rg("hwf")])
                S.dma("sp", wi[:], WIv[l][:, :, O_BI:O_BI + 512], reads=[WR], writes=[rg("hwi")])
                S.dma("sp", wg[:], WIv[l][:, :, O_BG:O_BG + 512], reads=[WR], writes=[rg("hwg")])
                YBv = YB.rearrange("(k p) t -> p k t", p=128)
                cmask = SB(st, "cmask", [128, 4, 512], BF16)
                S.op("dve", lambda h: h.memset(cmask[:], 0.0), writes=[rg("cmask")])
                for cc_ in range(4):
                    for hh in range(4):
                        S.op("dve", lambda h, cc_=cc_, hh=hh: h.memset(cmask[:, cc_, hh * 128 + cc_ * 32:hh * 128 + (cc_ + 1) * 32], 1.0), writes=[rg("cmask")])

                def proj(w_ap, wreg):
                    p, pr = ps()
                    for kc in range(8):
                        S.op("pe", lambda h, p=p, kc=kc: h.matmul(p[:, :], lhsT=ut[:, kc, :], rhs=w_ap[:, kc, :], start=(kc == 0), stop=(kc == 7)),
                             reads=[rg("hut"), wreg], writes=[pr])
                    return p, pr

                def chunk(c, d, outputs):
                    S.dma("sp", ut[:], UTv[:, :, c * 128:(c + 1) * 128], reads=[rg("UT")], writes=[rg("hut")])
                    pf, pfr = proj(wf[:, d], rg("hwf"))
                    S.op("act", lambda h: h.activation(out=f_t[:], in_=pf[:], func=AF.Sigmoid), reads=[pfr], writes=[rg("f_t")])
                    S.op("dve", lambda h: h.tensor_tensor(out=f_t[:], in0=f_t[:], in1=omb[:, d, :], op=ALU.mult), reads=[rg("f_t"), rg("omb")], writes=[rg("f_t")])
                    S.op("dve", lambda h: h.tensor_tensor(out=f_t[:], in0=f_t[:], in1=lbb[:, d, :], op=ALU.add), reads=[rg("f_t"), rg("lbb")], writes=[rg("f_t")])
                    S.op("act", lambda h: h.activation(out=g_t[:], in_=f_t[:], func=AF.Ln), reads=[rg("f_t")], writes=[rg("g_t")])
                    S.op("dve", lambda h: h.tensor_scalar(out=k_t[:], in0=f_t[:], scalar1=-1.0, scalar2=1.0, op0=ALU.mult, op1=ALU.add), reads=[rg("f_t")], writes=[rg("k_t")])
                    pd, pdr = ps()
                    S.op("pe", lambda h: h.matmul(pd[:, :], lhsT=C[:, 3 + d, :], rhs=g_t[:], start=True, stop=True), reads=[rg("C"), rg("g_t")], writes=[pdr])
                    S.op("act", lambda h: h.activation(out=eM[:], in_=pd[:], func=AF.Exp, scale=-1.0), reads=[pdr], writes=[rg("eM")])
                    if outputs:
                        S.op("act", lambda h: h.activation(out=eP[:], in_=pd[:], func=AF.Exp), reads=[pdr], writes=[rg("eP")])
                    pl, plr = ps()
                    for hh in range(4):
                        S.op("pe", lambda h, hh=hh: h.matmul(pl[:, 5 * hh:5 * hh + 5], lhsT=g_t[:, hh * 128:(hh + 1) * 128], rhs=C[:, 7, 0:5], start=True, stop=True),
                             reads=[rg("g_t"), rg("C")], writes=[plr])
                    S.op("dve", lambda h: h.tensor_tensor(out=Dl[:], in0=Dl[:], in1=pl[:, 0:20].rearrange("p (a b) -> p a b", b=5)[:, :, 4], op=ALU.add),
                         reads=[plr, rg("Dl")], writes=[rg("Dl")])
                    S.op("act", lambda h: h.activation(out=el[:].rearrange("p a b -> p (a b)"), in_=pl[:, 0:20], func=AF.Exp), reads=[plr], writes=[rg("el")])
                    for cc_ in range(4):
                        S.op("dve", lambda h, cc_=cc_: h.scalar_tensor_tensor(out=kb4[:, cc_, :], in0=k_t[:], scalar=C[:, 7, 8 + cc_:9 + cc_], in1=eM[:], op0=ALU.mult, op1=ALU.mult),
                             reads=[rg("k_t"), rg("eM"), rg("C")], writes=[rg("kb4")])
                    pv, pvr = proj(wi, rg("hwi"))
                    S.op("act", lambda h: h.activation(out=vb[:], in_=pv[:], func=AF.Copy), reads=[pvr], writes=[rg("vb")])
                    if outputs:
                        pq, pqr = proj(wq, rg("hwq"))
                        S.op("act", lambda h: h.activation(out=q_t[:], in_=pq[:], func=AF.Silu), reads=[pqr], writes=[rg("q_t")])
                        S.op("dve", lambda h: h.tensor_tensor(out=qb[:], in0=q_t[:], in1=eP[:], op=ALU.mult), reads=[rg("q_t"), rg("eP")], writes=[rg("qb")])
                        S.op("dve", lambda h: h.tensor_tensor(out=kb[:], in0=k_t[:], in1=eM[:], op=ALU.mult), reads=[rg("k_t"), rg("eM")], writes=[rg("kb")])
                        pt, ptr = ps()
                        ptb = pt[:]
                        for hh in range(4):
                            S.op("pe", lambda h, hh=hh, ptb=ptb: h.transpose(ptb[:, hh * 128:(hh + 1) * 128], qb[:, hh * 128:(hh + 1) * 128], ident), reads=[rg("qb"), rg("C")], writes=[ptr])
                        S.op("act", lambda h, ptb=ptb: h.activation(out=qT[:], in_=ptb[:, 0:512], func=AF.Copy), reads=[ptr], writes=[rg("qT")])
                        for cc_ in range(4):
                            S.op("dve", lambda h, cc_=cc_: h.tensor_tensor(out=qT4[:, cc_, :], in0=qT[:], in1=cmask[:, cc_, :], op=ALU.mult),
                                 reads=[rg("qT"), rg("cmask")], writes=[rg("qT4")])
                        pt2, ptr2 = ps()
                        ptb2 = pt2[:]
                        for hh in range(4):
                            S.op("pe", lambda h, hh=hh, ptb2=ptb2: h.transpose(ptb2[:, hh * 128:(hh + 1) * 128], kb[:, hh * 128:(hh + 1) * 128], ident), reads=[rg("kb"), rg("C")], writes=[ptr2])
                        S.op("act", lambda h, ptb2=ptb2: h.activation(out=kT[:], in_=ptb2[:, 0:512], func=AF.Copy), reads=[ptr2], writes=[rg("kT")])
                        psc, pscr = ps()
                        for hh in range(4):
                            S.op("pe", lambda h, hh=hh: h.matmul(psc[:, hh * 128:(hh + 1) * 128], lhsT=kT[:, hh * 128:(hh + 1) * 128], rhs=qT[:, hh * 128:(hh + 1) * 128],
                                                                 start=True, stop=True), reads=[rg("kT"), rg("qT")], writes=[pscr])
                        for hh in range(4):
                            S.op("dve", lambda h, hh=hh: h.tensor_tensor(out=scT[:, hh * 128:(hh + 1) * 128], in0=psc[:, hh * 128:(hh + 1) * 128], in1=C[:, 3 + d, :], op=ALU.mult),
                                 reads=[pscr, rg("C")], writes=[rg("scT")])
                    order = [0, 1, 2, 3] if d == 0 else [3, 2, 1, 0]
                    pps = []
                    for cc_ in range(4):
                        pp, ppr = ps()
                        for hh in range(4):
                            sl = slice(hh * 128, (hh + 1) * 128)
                            S.op("pe", lambda h, sl=sl, pp=pp, cc_=cc_: h.matmul(pp[:, sl], lhsT=kb4[:, cc_, sl], rhs=vb[:, sl], start=True, stop=True), reads=[rg("kb4"), rg("vb")], writes=[ppr])
                        pps.append((pp, ppr))
                    for cc_ in order:
                        pp, ppr = pps[cc_]
                        if outputs:
                            S.op("act", lambda h, cc_=cc_: h.activation(out=Tb4[:, cc_, :], in_=Ss[:], func=AF.Copy), reads=[rg("Ss")], writes=[rg("Tb4")])
                        S.op("dve", lambda h, pp=pp: h.tensor_tensor(out=Tf[:], in0=Ss[:], in1=pp[:], op=ALU.add), reads=[rg("Ss"), ppr], writes=[rg("Tf")])
                        for hh in range(4):
                            S.op("dve", lambda h, hh=hh, cc_=cc_: h.tensor_scalar(out=Ss[:, hh * 128:(hh + 1) * 128], in0=Tf[:, hh * 128:(hh + 1) * 128],
                                                                                scalar1=el[:, hh, cc_:cc_ + 1], scalar2=None, op0=ALU.mult),
                                 reads=[rg("Tf"), rg("el")], writes=[rg("Ss")])
                    if not outputs:
                        return
                    po, por = ps()
                    for hh in range(4):
                        sl = slice(hh * 128, (hh + 1) * 128)
                        S.op("pe", lambda h, sl=sl: h.matmul(po[:, sl], lhsT=scT[:, sl], rhs=vb[:, sl], start=True, stop=False), reads=[rg("scT"), rg("vb")], writes=[por])
                        for cc_ in range(4):
                            S.op("pe", lambda h, sl=sl, cc_=cc_: h.matmul(po[:, sl], lhsT=qT4[:, cc_, sl], rhs=Tb4[:, cc_, sl], start=False, stop=(cc_ == 3)), reads=[rg("qT4"), rg("Tb4")], writes=[por])
                    if d == 0:
                        S.op("act", lambda h: h.activation(out=o_t[:], in_=po[:], func=AF.Copy), reads=[por], writes=[rg("o_t")])
                        S.dma("sp", OF[c * 128:(c + 1) * 128, :], o_t[:], reads=[rg("o_t")], writes=[rg("OF")])
                        return
                    S.dma("sp", of_t[:], OF[c * 128:(c + 1) * 128, :], reads=[rg("OF")], writes=[rg("of_t")])
                    S.op("dve", lambda h: h.tensor_tensor(out=o_t[:], in0=of_t[:], in1=po[:], op=ALU.add), reads=[rg("of_t"), por], writes=[rg("o_t")])
                    S.op("dve", lambda h: h.memset(oss[:], 0.0), writes=[rg("oss")])
                    for hh in range(4):
                        S.op("act", lambda h, hh=hh: h.activation(out=junk[:], in_=o_t[:, hh * 128:(hh + 1) * 128], func=AF.Square, accum_out=oss[:, hh:hh + 1]),
                             reads=[rg("o_t"), rg("oss")], writes=[rg("hjunk"), rg("oss")])
                    rstd_from_ss(oss[:], 4, 1.0 / 128, rg("oss"))
                    pg, pgr = proj(wg, rg("hwg"))
                    S.op("act", lambda h: h.activation(out=zg[:], in_=pg[:], func=AF.Silu), reads=[pgr], writes=[rg("zg")])
                    S.op("dve", lambda h: h.tensor_tensor(out=zg[:], in0=zg[:], in1=hgb[:], op=ALU.mult), reads=[rg("zg"), rg("hgb")], writes=[rg("zg")])
                    for hh in range(4):
                        sl = slice(hh * 128, (hh + 1) * 128)
                        S.op("dve", lambda h, hh=hh, sl=sl: h.scalar_tensor_tensor(out=yb[:, sl], in0=o_t[:, sl], scalar=oss[:, hh:hh + 1], in1=zg[:, sl], op0=ALU.mult, op1=ALU.mult),
                             reads=[rg("o_t"), rg("oss"), rg("zg")], writes=[rg("yb")])
                    pt, ptr = ps()
                    ptb = pt[:]
                    for hh in range(4):
                        S.op("pe", lambda h, hh=hh, ptb=ptb: h.transpose(ptb[:, hh * 128:(hh + 1) * 128], yb[:, hh * 128:(hh + 1) * 128], ident), reads=[rg("yb"), rg("C")], writes=[ptr])
                    S.op("act", lambda h, ptb=ptb: h.activation(out=ybT[:].rearrange("p a b -> p (a b)"), in_=ptb[:, 0:512], func=AF.Copy), reads=[ptr], writes=[rg("ybT")])
                    S.dma("sp", YBv[:, :, c * 128:(c + 1) * 128], ybT[:], reads=[rg("ybT")], writes=[rg("YB")])

                ctx_out = full and l == 0
                for d in range(2):
                    S.op("dve", lambda h: h.memset(Ss[:], 0.0), writes=[rg("Ss")])
                    S.op("dve", lambda h: h.memset(Dl[:], 0.0), writes=[rg("Dl")])
                    order = [0, 1] if d == 0 else [1, 0]
                    if (not full) or ctx_out:
                        for c in order:
                            chunk(c, d, ctx_out)
                        if not full:
                            S.op("dve", lambda h, d=d: h.tensor_copy(out=sctx[:, d, :], in_=Ss[:]), reads=[rg("Ss")], writes=[rg("sctx")])
                    if full:
                        S.op("dve", lambda h, d=d: h.tensor_copy(out=Ss[:], in_=scin[:, d, :]), reads=[rg("scin")], writes=[rg("Ss")])
                    else:
                        S.op("dve", lambda h: h.memset(Ss[:], 0.0), writes=[rg("Ss")])
                        S.op("dve", lambda h: h.memset(Dl[:], 0.0), writes=[rg("Dl")])
                    lat = list(range(2, NCH))
                    if d == 1:
                        lat = lat[::-1]
                    for c in lat:
                        chunk(c, d, full)
                    if not full:
                        S.dma("sp", SX[:, d * 512:(d + 1) * 512], Ss[:], reads=[rg("Ss")], writes=[rg("SX")])
                        S.dma("sp", SX[:, 1024 + 4 * d:1028 + 4 * d], Dl[:], reads=[rg("Dl")], writes=[rg("SX")])
                S.barrier()

        def exchange(l):
            import os as _os
            if mode == "B":
                pass
            elif _os.environ.get("NOCC"):
                for r_ in range(4):
                    S.dma("sp", KG[r_ * 128:(r_ + 1) * 128, :], KX, reads=[rg("KX")], writes=[rg("KG")])
                    S.dma("sp", VG[r_ * T:(r_ + 1) * T, :], VX, reads=[rg("VX")], writes=[rg("VG")])
                    S.dma("sp", SG[r_ * 128:(r_ + 1) * 128, :], SX, reads=[rg("SX")], writes=[rg("SG")])
            else:
                S.cc(lambda h: h.collective_compute("AllGather", ALU.bypass, replica_groups=GROUPS, ins=[KX], outs=[KG]), reads=[rg("KX")], writes=[rg("KG")])
                S.cc(lambda h: h.collective_compute("AllGather", ALU.bypass, replica_groups=GROUPS, ins=[VX], outs=[VG]), reads=[rg("VX")], writes=[rg("VG")])
                S.cc(lambda h: h.collective_compute("AllGather", ALU.bypass, replica_groups=GROUPS, ins=[SX], outs=[SG]), reads=[rg("SX")], writes=[rg("SG")])
            S.barrier()
            with contextlib.ExitStack() as st:
                sg = SB(st, "sg", [128, 4, 1032], F32)
                ed = SB(st, "ed", [128, 4], F32)
                S.dma("sp", sg[:], SG.rearrange("(r p) n -> p r n", p=128), reads=[rg("SG")], writes=[rg("sg")])
                for d in range(2):
                    S.op("dve", lambda h, d=d: h.tensor_copy(out=scin[:, d, :], in_=sctx[:, d, :]), reads=[rg("sctx")], writes=[rg("scin")])
                    order = [0, 1, 2, 3] if d == 0 else [3, 2, 1, 0]
                    for r in order:
                        mcol = mskt[:, 4 * d + r:4 * d + r + 1]
                        S.op("dve", lambda h, d=d, r=r, mcol=mcol: h.tensor_scalar(out=ed[:], in0=sg[:, r, 1024 + 4 * d:1028 + 4 * d], scalar1=mcol, scalar2=None, op0=ALU.mult),
                             reads=[rg("sg"), rg("mskt")], writes=[rg("ed")])
                        S.op("act", lambda h: h.activation(out=ed[:], in_=ed[:], func=AF.Exp), reads=[rg("ed")], writes=[rg("ed")])
                        for hh in range(4):
                            sl = slice(hh * 128, (hh + 1) * 128)
                            S.op("dve", lambda h, d=d, hh=hh, sl=sl: h.tensor_scalar(out=scin[:, d, sl], in0=scin[:, d, sl], scalar1=ed[:, hh:hh + 1], scalar2=None, op0=ALU.mult),
                                 reads=[rg("scin"), rg("ed")], writes=[rg("scin")])
                        S.op("dve", lambda h, d=d, r=r, mcol=mcol: h.scalar_tensor_tensor(out=scin[:, d, :], in0=sg[:, r, d * 512:(d + 1) * 512], scalar=mcol, in1=scin[:, d, :],
                                                                                       op0=ALU.mult, op1=ALU.add),
                             reads=[rg("sg"), rg("mskt"), rg("scin")], writes=[rg("scin")])
                S.barrier()

        def sweep_conv(l):
            with contextlib.ExitStack() as st:
                wv_ = SB(st, "cwv", [128, 8, 512], BF16)
                wb_ = SB(st, "cwb", [128, 8, 512], BF16)
                wc_ = SB(st, "cwc", [128, 8, 512], BF16)
                cw = SB(st, "cw", [128, 4, 3], F32)
                uw = SB(st, "uw", [128, 8, 258], BF16)
                hal = SB(st, "hal", [128, 4, 16], BF16)
                halc = SB(st, "halc", [128, 16], F32)
                hsum = SB(st, "hsum", [128, 16], F32)
                pbuf = SB(st, "pbuf", [128, 258], F32)
                zc = SB(st, "zc", [128, 258], F32)
                y = SB(st, "cy", [128, 256], F32)
                pa = SB(st, "cpa", [128, 4, 256], BF16)
                WR = rg("WIb%d" % l)
                S.dma("sp", wv_[:], WIv[l][:, :, O_AVAL:O_AVAL + 512], reads=[WR], writes=[rg("cwv")])
                S.dma("sp", wb_[:], WIv[l][:, :, O_AB:O_AB + 512], reads=[WR], writes=[rg("cwb")])
                S.dma("sp", wc_[:], WIv[l][:, :, O_AC:O_AC + 512], reads=[WR], writes=[rg("cwc")])
                S.dma("sp", cw[:], conv_w[wl(l)], writes=[rg("cw")])
                S.dma("sp", hal[:], KG.rearrange("(r p) n -> p r n", p=128)[:, :, T:T + 16], reads=[rg("KG")], writes=[rg("hal")])
                S.op("dve", lambda h: h.memset(hsum[:], 0.0), writes=[rg("hsum")])
                for r in range(4):
                    S.op("dve", lambda h, r=r: h.tensor_copy(out=halc[:], in_=hal[:, r, :]), reads=[rg("hal")], writes=[rg("halc")])
                    S.op("dve", lambda h, r=r: h.scalar_tensor_tensor(out=hsum[:, 0:8], in0=halc[:, 8:16], scalar=mskt[:, 8 + r:9 + r], in1=hsum[:, 0:8], op0=ALU.mult, op1=ALU.add),
                         reads=[rg("halc"), rg("mskt"), rg("hsum")], writes=[rg("hsum")])
                    S.op("dve", lambda h, r=r: h.scalar_tensor_tensor(out=hsum[:, 8:16], in0=halc[:, 0:8], scalar=mskt[:, 12 + r:13 + r], in1=hsum[:, 8:16], op0=ALU.mult, op1=ALU.add),
                         reads=[rg("halc"), rg("mskt"), rg("hsum")], writes=[rg("hsum")])
                PAv = PA.rearrange("(k p) t -> p k t", p=128)
                ntl = TT // 256
                for tl in range(ntl):
                    if tl == 0 and l == 1:
                        continue
                    t0 = tl * 256
                    lo_in = (tl >= 2)
                    hi_in = (tl >= 1 and tl < ntl - 1)
                    c0 = t0 - 1 if lo_in else t0
                    c1 = t0 + 257 if hi_in else t0 + 256
                    S.dma("sp", uw[:, :, (c0 - t0 + 1):(c1 - t0 + 1)], UTv[:, :, c0:c1], reads=[rg("UT")], writes=[rg("uw")])
                    if not lo_in:
                        if tl == 0:
                            S.op("dve", lambda h: h.memset(uw[:, :, 0:1], 0.0), writes=[rg("uw")])
                        else:
                            S.op("dve", lambda h: h.tensor_copy(out=uw[:, :, 0], in_=hsum[:, 0:8]), reads=[rg("hsum")], writes=[rg("uw")])
                    if not hi_in:
                        if tl == 0:
                            S.op("dve", lambda h: h.memset(uw[:, :, 257:258], 0.0), writes=[rg("uw")])
                        else:
                            S.op("dve", lambda h: h.tensor_copy(out=uw[:, :, 257], in_=hsum[:, 8:16]), reads=[rg("hsum")], writes=[rg("uw")])
                    for ch in range(4):
                        cs_ = slice(ch * 128, (ch + 1) * 128)
                        p1, p1r = ps()
                        p2, p2r = ps()
                        p3, p3r = ps()
                        for kc in range(8):
                            S.op("pe", lambda h, p1=p1, kc=kc, cs_=cs_: h.matmul(p1[:, 0:258], lhsT=wv_[:, kc, cs_], rhs=uw[:, kc, :], start=(kc == 0), stop=(kc == 7)),
                                 reads=[rg("cwv"), rg("uw")], writes=[p1r])
                        for kc in range(8):
                            S.op("pe", lambda h, p2=p2, kc=kc, cs_=cs_: h.matmul(p2[:, 0:258], lhsT=wc_[:, kc, cs_], rhs=uw[:, kc, :], start=(kc == 0), stop=(kc == 7)),
                                 reads=[rg("cwc"), rg("uw")], writes=[p2r])
                        for kc in range(8):
                            S.op("pe", lambda h, p3=p3, kc=kc, cs_=cs_: h.matmul(p3[:, 0:256], lhsT=wb_[:, kc, cs_], rhs=uw[:, kc, 1:257], start=(kc == 0), stop=(kc == 7)),
                                 reads=[rg("cwb"), rg("uw")], writes=[p3r])
                        S.op("act", lambda h, p2=p2: h.activation(out=zc[:], in_=p2[:, 0:258], func=AF.Copy), reads=[p2r], writes=[rg("zc")])
                        S.op("dve", lambda h, p1=p1: h.tensor_tensor(out=pbuf[:], in0=p1[:, 0:258], in1=zc[:], op=ALU.mult), reads=[p1r, rg("zc")], writes=[rg("pbuf")])
                        S.op("dve", lambda h, ch=ch: h.tensor_scalar(out=y[:], in0=pbuf[:, 0:256], scalar1=cw[:, ch, 0:1], scalar2=None, op0=ALU.mult),
                             reads=[rg("pbuf"), rg("cw")], writes=[rg("cy")])
                        S.op("dve", lambda h, ch=ch: h.scalar_tensor_tensor(out=y[:], in0=pbuf[:, 1:257], scalar=cw[:, ch, 1:2], in1=y[:], op0=ALU.mult, op1=ALU.add),
                             reads=[rg("pbuf"), rg("cw"), rg("cy")], writes=[rg("cy")])
                        S.op("dve", lambda h, ch=ch: h.scalar_tensor_tensor(out=y[:], in0=pbuf[:, 2:258], scalar=cw[:, ch, 2:3], in1=y[:], op0=ALU.mult, op1=ALU.add),
                             reads=[rg("pbuf"), rg("cw"), rg("cy")], writes=[rg("cy")])
                        S.op("dve", lambda h, p3=p3, ch=ch: h.tensor_tensor(out=pa[:, ch, :], in0=p3[:, 0:256], in1=y[:], op=ALU.mult), reads=[p3r, rg("cy")], writes=[rg("cpa")])
                    S.dma("sp", PAv[:, :, t0:t0 + 256], pa[:], reads=[rg("cpa")], writes=[rg("PA")])
                S.barrier()

        def sweep_att(l):
            with contextlib.ExitStack() as st:
                wq = SB(st, "awq", [128, 8, 512], BF16)
                gq = SB(st, "gq", [64, 1], F32)
                ut = SB(st, "aut", [128, 8, 512], BF16)
                bufs = tuple(SB(st, n, [64, 512], F32) for n in ("xq", "sq", "rs", "xn", "t1"))
                cs = SB(st, "acs", [64, 2, 512], F32)
                QT = SB(st, "QT", [64, 8, 512], BF16)
                Kb = [SB(st, "Kb%d" % i, [64, 512], BF16) for i in range(3)]
                Vb = [SB(st, "Vb%d" % i, [128, 4, 65], BF16) for i in range(3)]
                pt = [SB(st, "pt%d" % i, [128, 512], BF16) for i in range(4)]
                lr = SB(st, "lr", [128, 512], F32)
                lb_ = SB(st, "lb_", [64, 512], F32)
                yc = SB(st, "ayc", [64, 512], BF16)
                WR = rg("WIb%d" % l)
                S.dma("sp", wq[:], WIv[l][:, :, O_CQ:O_CQ + 512], reads=[WR], writes=[rg("awq")])
                S.dma("sp", gq[:], qng[wl(l)], writes=[rg("gq")])
                kbn = [0]
                ptn = [0]
                qtiles = []
                if l == 0:
                    qtiles.append((0, 256, True))
                for i in range(T // 512):
                    qtiles.append((CT + i * 512, 512, False))
                def _att_tile(t0, n, isctx):
                    S.dma("sp", ut[:, :, 0:n], UTv[:, :, t0:t0 + n], reads=[rg("UT")], writes=[rg("aut")])
                    if not isctx:
                        S.dma("sp", cs[:, 0, :], cosT[:, t0 - CT:t0 - CT + 512], writes=[rg("rope")])
                        S.dma("sp", cs[:, 1, :], sinT[:, t0 - CT:t0 - CT + 512], writes=[rg("rope")])
                    for hd in range(8):
                        p, pr = PS[4 + hd % 4], PSR[4 + hd % 4]
                        for kc in range(8):
                            S.op("pe", lambda h, p=p, kc=kc, hd=hd: h.matmul(p[0:64, 0:n], lhsT=wq[:, kc, hd * 64:(hd + 1) * 64], rhs=ut[:, kc, 0:n], start=(kc == 0), stop=(kc == 7)),
                                 reads=[rg("awq"), rg("aut")], writes=[pr])
                        qk_norm_rope(bufs, p[0:64, 0:n], pr, n, gq[:, 0:1], rg("gq"), None if isctx else cs[:, 0, 0:n], None if isctx else cs[:, 1, 0:n],
                                     QT[:, hd, 0:n], rg("QT"))
                    blocks = [("c", 0, 0, 256)]
                    if not isctx:
                        for r in range(4):
                            for kb0 in range(0, T, 512):
                                blocks.append(("g", r, kb0, 512))
                    for kvh in range(2):
                        accs = [(PS[i], PSR[i]) for i in range(4)]
                        for bi, (kind, r, k0, nk) in enumerate(blocks):
                            slot = kbn[0] % 3
                            kbn[0] += 1
                            kbt, vbt = Kb[slot], Vb[slot]
                            kreg, vreg = rg("Kb%d" % slot), rg("Vb%d" % slot)
                            if kind == "c":
                                S.dma("sp", kbt[:, 0:nk], KC[kvh * 64:(kvh + 1) * 64, 0:nk], reads=[rg("KC")], writes=[kreg])
                                S.dma("sp", vbt[:, 0:nk // 128, :], VC[0:nk, :].rearrange("(kt p) c -> p kt c", p=128)[:, :, kvh * 65:(kvh + 1) * 65],
                                      reads=[rg("VC")], writes=[vreg])
                            else:
                                S.dma("sp", kbt[:, 0:nk], KG[r * 128 + kvh * 64:r * 128 + (kvh + 1) * 64, k0:k0 + nk], reads=[rg("KG")], writes=[kreg])
                                S.dma("sp", vbt[:, 0:nk // 128, :], VG[r * T + k0:r * T + k0 + nk, :].rearrange("(kt p) c -> p kt c", p=128)[:, :, kvh * 65:(kvh + 1) * 65],
                                      reads=[rg("VG")], writes=[vreg])
                            for hh in range(4):
                                hd = kvh * 4 + hh
                                acc, accr = accs[hh]
                                for kt in range(nk // 128):
                                    sp_, spr = PS[4 + ptn[0] % 4], PSR[4 + ptn[0] % 4]
                                    ptt, ptr_ = pt[ptn[0] % 4], rg("pt%d" % (ptn[0] % 4))
                                    ptn[0] += 1
                                    S.op("pe", lambda h, sp_=sp_, kbt=kbt, kt=kt, hd=hd: h.matmul(sp_[:, 0:n], lhsT=kbt[:, kt * 128:(kt + 1) * 128], rhs=QT[:, hd, 0:n], start=True, stop=True),
                                         reads=[kreg, rg("QT")], writes=[spr])
                                    S.op("act", lambda h, sp_=sp_, ptt=ptt: h.activation(out=ptt[:, 0:n], in_=sp_[:, 0:n], func=AF.Exp, scale=0.125), reads=[spr], writes=[ptr_])
                                    first = (bi == 0 and kt == 0)
                                    last = (bi == len(blocks) - 1 and kt == nk // 128 - 1)
                                    S.op("pe", lambda h, acc=acc, vbt=vbt, kt=kt, ptt=ptt, first=first, last=last: h.matmul(acc[0:65, 0:n], lhsT=vbt[:, kt, :], rhs=ptt[:, 0:n], start=first, stop=last),
                                         reads=[vreg, ptr_], writes=[accr])
                        for hh in range(4):
                            hd = kvh * 4 + hh
                            acc, accr = accs[hh]
                            S.op("dve", lambda h, acc=acc: h.reciprocal(out=lr[64:65, 0:n], in_=acc[64:65, 0:n]), reads=[accr], writes=[rg("lr")])
                            pb, pbr = PS[4 + hh], PSR[4 + hh]
                            S.op("pe", lambda h, pb=pb: h.matmul(pb[0:64, 0:n], lhsT=C[64:65, 5, 0:64], rhs=lr[64:65, 0:n], start=True, stop=True), reads=[rg("C"), rg("lr")], writes=[pbr])
                            S.op("act", lambda h, pb=pb: h.activation(out=lb_[:, 0:n], in_=pb[0:64, 0:n], func=AF.Copy), reads=[pbr], writes=[rg("lb_")])
                            S.op("dve", lambda h, acc=acc: h.tensor_tensor(out=yc[:, 0:n], in0=acc[0:64, 0:n], in1=lb_[:, 0:n], op=ALU.mult), reads=[accr, rg("lb_")], writes=[rg("ayc")])
                            S.dma("sp", YC[hd * 64:(hd + 1) * 64, t0:t0 + n], yc[:, 0:n], reads=[rg("ayc")], writes=[rg("YC")])
                for _a in qtiles:
                    _att_tile(*_a)
                S.barrier()

        def sweep_merge(l):
            with contextlib.ExitStack() as st:
                wgt = SB(st, "mwg", [128, 8, 3072], BF16)
                wa = SB(st, "mwa", [128, 4, D], BF16)
                wb_ = SB(st, "mwb", [128, 4, D], BF16)
                wc_ = SB(st, "mwc", [64, 8, D], BF16)
                wo = SB(st, "mwo", [128, 8, D], BF16)
                ut = SB(st, "mut", [128, 8, 512], BF16)
                pa = SB(st, "mpa", [128, 4, 512], BF16)
                yb = SB(st, "myb", [128, 4, 512], BF16)
                yc = SB(st, "myc", [64, 8, 512], BF16)
                sg = SB(st, "msg", [128, 512], F32)
                tm = SB(st, "mtm", [128, 512], F32)
                tm2 = SB(st, "mtm2", [128, 512], F32)
                mT = SB(st, "mT", [128, 8, 512], BF16)
                hrow = SB(st, "hrow", [128, D], F32)
                hnew = SB(st, "hnew", [128, D], F32)
                WR = rg("WIb%d" % l)
                S.dma("sp", wgt[:], WIv[l][:, :, O_GA:O_GA + 3072], reads=[WR], writes=[rg("mwg")])
                S.dma("pool", wa[:], w_a[wl(l)].rearrange("(k p) n -> p k n", p=128), writes=[rg("mwa")])
                S.dma("pool", wb_[:], w_b[wl(l)].rearrange("(k p) n -> p k n", p=128), writes=[rg("mwb")])
                S.dma("pool", wc_[:], w_c[wl(l)].rearrange("(h p) n -> p h n", p=64), writes=[rg("mwc")])
                S.dma("pool", wo[:], w_o[wl(l)].rearrange("(k p) n -> p k n", p=128), writes=[rg("mwo")])
                PAv = PA.rearrange("(k p) t -> p k t", p=128)
                YBv = YB.rearrange("(k p) t -> p k t", p=128)
                YCv = YC.rearrange("(h p) t -> p h t", p=64)
                tiles = []
                if l == 0:
                    tiles.append((0, 256, 1))
                for i in range(T // 512):
                    tiles.append((CT + i * 512, 512, 0))
                def _mrg_tile(t0, n, which):
                    S.dma("sp", ut[:, :, 0:n], UTv[:, :, t0:t0 + n], reads=[rg("UT")], writes=[rg("mut")])
                    S.dma("sp", pa[:, :, 0:n], PAv[:, :, t0:t0 + n], reads=[rg("PA")], writes=[rg("mpa")])
                    S.dma("sp", yb[:, :, 0:n], YBv[:, :, t0:t0 + n], reads=[rg("YB")], writes=[rg("myb")])
                    S.dma("sp", yc[:, :, 0:n], YCv[:, :, t0:t0 + n], reads=[rg("YC")], writes=[rg("myc")])
                    for ccn in range(8):
                        cs_ = slice(ccn * 128, (ccn + 1) * 128)
                        ys = []
                        p, pr = ps()
                        for k in range(4):
                            S.op("pe", lambda h, p=p, k=k, cs_=cs_: h.matmul(p[:, 0:n], lhsT=wa[:, k, cs_], rhs=pa[:, k, 0:n], start=(k == 0), stop=(k == 3)), reads=[rg("mwa"), rg("mpa")], writes=[pr])
                        ys.append((p, pr))
                        p, pr = ps()
                        for k in range(4):
                            S.op("pe", lambda h, p=p, k=k, cs_=cs_: h.matmul(p[:, 0:n], lhsT=wb_[:, k, cs_], rhs=yb[:, k, 0:n], start=(k == 0), stop=(k == 3)), reads=[rg("mwb"), rg("myb")], writes=[pr])
                        ys.append((p, pr))
                        p, pr = ps()
                        for k in range(8):
                            S.op("pe", lambda h, p=p, k=k, cs_=cs_: h.matmul(p[:, 0:n], lhsT=wc_[:, k, cs_], rhs=yc[:, k, 0:n], start=(k == 0), stop=(k == 7)), reads=[rg("mwc"), rg("myc")], writes=[pr])
                        ys.append((p, pr))
                        for br in range(3):
                            p, pr = ps()
                            for kc in range(8):
                                S.op("pe", lambda h, p=p, kc=kc, br=br, ccn=ccn: h.matmul(p[:, 0:n], lhsT=wgt[:, kc, br * 1024 + ccn * 128:br * 1024 + (ccn + 1) * 128], rhs=ut[:, kc, 0:n],
                                                                                       start=(kc == 0), stop=(kc == 7)), reads=[rg("mwg"), rg("mut")], writes=[pr])
                            S.op("act", lambda h, p=p: h.activation(out=sg[:, 0:n], in_=p[:, 0:n], func=AF.Sigmoid), reads=[pr], writes=[rg("msg")])
                            yp, ypr = ys[br]
                            dst = tm if br == 0 else tm2
                            dstn = "mtm" if br == 0 else "mtm2"
                            S.op("dve", lambda h, yp=yp, dst=dst: h.tensor_tensor(out=dst[:, 0:n], in0=yp[:, 0:n], in1=sg[:, 0:n], op=ALU.mult), reads=[ypr, rg("msg")], writes=[rg(dstn)])
                            if br == 1:
                                S.op("pool", lambda h: h.tensor_tensor(out=tm[:, 0:n], in0=tm[:, 0:n], in1=tm2[:, 0:n], op=ALU.add), reads=[rg("mtm"), rg("mtm2")], writes=[rg("mtm")])
                            if br == 2:
                                S.op("dve", lambda h, ccn=ccn: h.tensor_tensor(out=mT[:, ccn, 0:n], in0=tm[:, 0:n], in1=tm2[:, 0:n], op=ALU.add), reads=[rg("mtm"), rg("mtm2")], writes=[rg("mT")])
                    for sub in range(n // 128):
                        c = (t0 // 128) + sub
                        S.dma("sp", hrow[:], hsrc(l, c), writes=[rg("hrow")])
                        for half in range(2):
                            p, pr = ps()
                            for kc in range(8):
                                S.op("pe", lambda h, p=p, kc=kc, sub=sub, half=half: h.matmul(p[:, :], lhsT=mT[:, kc, sub * 128:(sub + 1) * 128], rhs=wo[:, kc, half * 512:(half + 1) * 512],
                                                                                            start=(kc == 0), stop=(kc == 7)), reads=[rg("mT"), rg("mwo")], writes=[pr])
                            hs = slice(half * 512, (half + 1) * 512)
                            S.op("dve", lambda h, p=p, hs=hs, which=which: h.tensor_tensor(out=hnew[:, hs], in0=p[:, :], in1=g1b[:, which, hs], op=ALU.mult), reads=[pr, rg("g1b")], writes=[rg("hnew")])
                            S.op("pool", lambda h, hs=hs: h.tensor_tensor(out=hnew[:, hs], in0=hnew[:, hs], in1=hrow[:, hs], op=ALU.add), reads=[rg("hnew"), rg("hrow")], writes=[rg("hnew")])
                        S.dma("sp", H1[c * 128:(c + 1) * 128, :], hnew[:], reads=[rg("hnew")], writes=[rg("H1")])
                for _a in tiles:
                    _mrg_tile(*_a)
                S.barrier()

        import os as _os2
        MOE_STOP = int(_os2.environ.get("MOE_STOP", "0"))

        def sweep_moe(l, last):
            with contextlib.ExitStack() as st:
                rwt = SB(st, "rwt", [128, 8, 16], F32)
                ht = SB(st, "ht", [128, D], F32)
                junk = SB(st, "junk", [128, D], BF16)
                ss = SB(st, "ss", [128, 4], F32)
                hn = SB(st, "hn", [128, D], F32)
                ub = SB(st, "ub", [128, 8, 128], BF16)
                etmp = SB(st, "etmp2", [128, D], F32)
                h1t = SB(st, "h1t", [128, 4, D], F32)
                mTb = SB(st, "mTb", [128, 8, 512], BF16)
                gw = SB(st, "gw", [128, 4, 16], F32)
                sc = SB(st, "rsc", [128, 16], F32)
                sel = SB(st, "rsel", [128, 16], F32)
                sel2 = SB(st, "rsel2", [128, 16], F32)
                eq = SB(st, "req", [128, 16], F32)
                m1 = SB(st, "rm1", [128, 4], F32)
                m2 = SB(st, "rm2", [128, 4], F32)
                gs = SB(st, "rgs", [128, 4], F32)
                gmx = SB(st, "rgmx", [128, 4], F32)
                pw = SB(st, "rpw", [128, 4], F32)
                W4 = [SB(st, "we%d" % i, [128, 8, D], BF16) for i in range(4)]
                sgt = SB(st, "esg", [128, 512], F32)
                hid = SB(st, "hid", [128, 8, 512], BF16)
                acc = SB(st, "eacc", [128, 4, D], F32)
                tmp = etmp
                S.dma("sp", rwt[:], rw.rearrange("(kc p) e -> p kc e", p=128), writes=[rg("rwt")])
                rwtb = SB(st, "rwtb", [128, 8, 16], BF16)
                S.op("act", lambda h: h.activation(out=rwtb[:], in_=rwt[:], func=AF.Copy), reads=[rg("rwt")], writes=[rg("rwtb")])
                tiles = []
                if l == 0:
                    tiles.append((0, 256, 1))
                for i in range(T // 512):
                    tiles.append((CT + i * 512, 512, 0))
                en = [0]
                def _moe_tile(t0, n, which):
                    nsub = n // 128
                    for sub in range(nsub):
                        c = t0 // 128 + sub
                        norm_chunk((h1t[:, sub, :], junk, ss, hn, mTb[:, :, sub * 128:(sub + 1) * 128], None), H1[c * 128:(c + 1) * 128, :], a2, 24, which, False)
                        if MOE_STOP == 1:
                            continue
                        p, pr = ps()
                        for kc in range(8):
                            S.op("pe", lambda h, p=p, kc=kc, sub=sub: h.matmul(p[:, 0:16], lhsT=mTb[:, kc, sub * 128:(sub + 1) * 128], rhs=rwtb[:, kc, :], start=(kc == 0), stop=(kc == 7)), reads=[rg("ub"), rg("rwtb")], writes=[pr])
                        S.op("act", lambda h, p=p: h.activation(out=sc[:], in_=p[:, 0:16], func=AF.Sigmoid), reads=[pr], writes=[rg("rsc")])
                        S.op("dve", lambda h: h.tensor_tensor(out=sel[:], in0=sc[:], in1=rbt[:], op=ALU.add), reads=[rg("rsc"), rg("rbt")], writes=[rg("rsel")])
                        def sv(i):
                            return sel[:].rearrange("p (g e) -> p g e", e=4)[:, :, i]
                        RS = [rg("rsel")]
                        S.op("dve", lambda h: h.tensor_tensor(out=m1[:], in0=sv(0), in1=sv(1), op=ALU.max), reads=RS, writes=[rg("rm1")])
                        S.op("dve", lambda h: h.tensor_tensor(out=pw[:], in0=sv(2), in1=sv(3), op=ALU.max), reads=RS, writes=[rg("rpw")])
                        S.op("dve", lambda h: h.tensor_tensor(out=m1[:], in0=m1[:], in1=pw[:], op=ALU.max), reads=[rg("rm1"), rg("rpw")], writes=[rg("rm1")])
                        for pi_, (i_, j_) in enumerate(((0, 1), (0, 2), (0, 3), (1, 2), (1, 3), (2, 3))):
                            if pi_ == 0:
                                S.op("dve", lambda h, i_=i_, j_=j_: h.tensor_tensor(out=m2[:], in0=sv(i_), in1=sv(j_), op=ALU.min), reads=RS, writes=[rg("rm2")])
                            else:
                                S.op("dve", lambda h, i_=i_, j_=j_: h.tensor_tensor(out=pw[:], in0=sv(i_), in1=sv(j_), op=ALU.min), reads=RS, writes=[rg("rpw")])
                                S.op("dve", lambda h: h.tensor_tensor(out=m2[:], in0=m2[:], in1=pw[:], op=ALU.max), reads=[rg("rm2"), rg("rpw")], writes=[rg("rm2")])
                        S.op("dve", lambda h: h.tensor_tensor(out=gs[:], in0=m1[:], in1=m2[:], op=ALU.add), reads=[rg("rm1"), rg("rm2")], writes=[rg("rgs")])
                        S.op("dve", lambda h: h.tensor_tensor(out=gmx[:, 0:1], in0=gs[:, 0:1], in1=gs[:, 1:2], op=ALU.max), reads=[rg("rgs")], writes=[rg("rgmx")])
                        S.op("dve", lambda h: h.tensor_tensor(out=gmx[:, 2:3], in0=gs[:, 2:3], in1=gs[:, 3:4], op=ALU.max), reads=[rg("rgs")], writes=[rg("rgmx")])
                        S.op("dve", lambda h: h.tensor_tensor(out=gmx[:, 0:1], in0=gmx[:, 0:1], in1=gmx[:, 2:3], op=ALU.max), reads=[rg("rgmx")], writes=[rg("rgmx")])
                        S.op("dve", lambda h: h.tensor_scalar(out=gs[:], in0=gs[:], scalar1=gmx[:, 0:1], scalar2=None, op0=ALU.is_ge), reads=[rg("rgs"), rg("rgmx")], writes=[rg("rgs")])
                        for g in range(4):
                            S.op("dve", lambda h, g=g: h.tensor_scalar(out=eq[:, g * 4:(g + 1) * 4], in0=sel[:, g * 4:(g + 1) * 4], scalar1=m2[:, g:g + 1], scalar2=gs[:, g:g + 1],
                                                                     op0=ALU.is_ge, op1=ALU.mult), reads=[rg("rsel"), rg("rm2"), rg("rgs")], writes=[rg("req")])
                        S.op("dve", lambda h: h.tensor_tensor(out=eq[:], in0=eq[:], in1=sc[:], op=ALU.mult), reads=[rg("req"), rg("rsc")], writes=[rg("req")])
                        S.op("dve", lambda h: h.memset(gmx[:, 1:2], 0.0), writes=[rg("rgmx")])
                        S.op("act", lambda h: h.activation(out=sel2[:], in_=eq[:], func=AF.Copy, accum_out=gmx[:, 1:2]), reads=[rg("req"), rg("rgmx")], writes=[rg("rsel2"), rg("rgmx")])
                        S.op("dve", lambda h: h.reciprocal(out=gmx[:, 1:2], in_=gmx[:, 1:2]), reads=[rg("rgmx")], writes=[rg("rgmx")])
                        S.op("dve", lambda h, sub=sub: h.tensor_scalar(out=gw[:, sub, :], in0=eq[:], scalar1=gmx[:, 1:2], scalar2=None, op0=ALU.mult), reads=[rg("req"), rg("rgmx")], writes=[rg("gw")])
                    if MOE_STOP in (1, 3):
                        return
                    for e in range(16):
                        slots = [(en[0] + k_) % 4 for k_ in range(3)]
                        en[0] += 3
                        wg_, wu_, wd_ = [W4[i_] for i_ in slots]
                        regs = [rg("we%d" % i_) for i_ in slots]
                        for k, (wt, src) in enumerate(((wg_, w_gate), (wu_, w_up), (wd_, w_down))):
                            S.dma("pool", wt[:], src[wl(l), e].rearrange("(kc p) n -> p kc n", p=128), writes=[regs[k]])
                        for ccn in range(8):
                            cs_ = slice(ccn * 128, (ccn + 1) * 128)
                            pg, pgr = ps()
                            pu, pur = ps()
                            for kc in range(8):
                                S.op("pe", lambda h, pg=pg, kc=kc, cs_=cs_, wg_=wg_: h.matmul(pg[:, 0:n], lhsT=wg_[:, kc, cs_], rhs=mTb[:, kc, 0:n], start=(kc == 0), stop=(kc == 7)), reads=[regs[0], rg("ub")], writes=[pgr])
                            for kc in range(8):
                                S.op("pe", lambda h, pu=pu, kc=kc, cs_=cs_, wu_=wu_: h.matmul(pu[:, 0:n], lhsT=wu_[:, kc, cs_], rhs=mTb[:, kc, 0:n], start=(kc == 0), stop=(kc == 7)), reads=[regs[1], rg("ub")], writes=[pur])
                            S.op("act", lambda h, pg=pg: h.activation(out=sgt[:, 0:n], in_=pg[:, 0:n], func=AF.Silu), reads=[pgr], writes=[rg("esg")])
                            S.op("dve", lambda h, pu=pu, ccn=ccn: h.tensor_tensor(out=hid[:, ccn, 0:n], in0=pu[:, 0:n], in1=sgt[:, 0:n], op=ALU.mult), reads=[pur, rg("esg")], writes=[rg("hid")])
                        for sub in range(nsub):
                            for half in range(2):
                                py, pyr = ps()
                                for kc in range(8):
                                    S.op("pe", lambda h, py=py, kc=kc, sub=sub, half=half, wd_=wd_: h.matmul(py[:, :], lhsT=hid[:, kc, sub * 128:(sub + 1) * 128], rhs=wd_[:, kc, half * 512:(half + 1) * 512],
                                                                                                          start=(kc == 0), stop=(kc == 7)), reads=[rg("hid"), regs[2]], writes=[pyr])
                                hs = slice(half * 512, (half + 1) * 512)
                                if e == 0:
                                    S.op("dve", lambda h, py=py, sub=sub, hs=hs, e=e: h.tensor_scalar(out=acc[:, sub, hs], in0=py[:, :], scalar1=gw[:, sub, e:e + 1], scalar2=None, op0=ALU.mult),
                                         reads=[pyr, rg("gw")], writes=[rg("eacc")])
                                else:
                                    S.op("dve", lambda h, py=py, sub=sub, hs=hs, e=e: h.scalar_tensor_tensor(out=acc[:, sub, hs], in0=py[:, :], scalar=gw[:, sub, e:e + 1], in1=acc[:, sub, hs], op0=ALU.mult, op1=ALU.add),
                                         reads=[pyr, rg("gw"), rg("eacc")], writes=[rg("eacc")])
                    for sub in range(nsub):
                        c = t0 // 128 + sub
                        S.op("dve", lambda h, sub=sub, which=which: h.tensor_tensor(out=tmp[:], in0=acc[:, sub, :], in1=g2b[:, which, :], op=ALU.mult), reads=[rg("eacc"), rg("g2b")], writes=[rg("ht")])
                        S.op("pool", lambda h, sub=sub: h.tensor_tensor(out=tmp[:], in0=tmp[:], in1=h1t[:, sub, :], op=ALU.add), reads=[rg("ht"), rg("ht")], writes=[rg("ht")])
                        if not last:
                            dst = HC[c * 128:(c + 1) * 128, :] if c < 2 else HL[(c - 2) * 128:(c - 1) * 128, :]
                            S.dma("sp", dst, tmp[:], reads=[rg("ht")], writes=[rg("HLC")])
                        else:
                            S.op("dve", lambda h: h.memset(ss[:, 1:2], 0.0), writes=[rg("ss")])
                            S.op("act", lambda h: h.activation(out=junk[:], in_=tmp[:], func=AF.Square, accum_out=ss[:, 1:2]), reads=[rg("ht"), rg("ss")], writes=[rg("junk"), rg("ss")])
                            rstd_from_ss(ss[:, 1:2], 1, 1.0 / D, rg("ss"))
                            S.op("dve", lambda h: h.scalar_tensor_tensor(out=hn[:], in0=tmp[:], scalar=ss[:, 1:2], in1=fgb[:], op0=ALU.mult, op1=ALU.mult), reads=[rg("ht"), rg("ss"), rg("fgb")], writes=[rg("hn")])
                            S.dma("sp", out[(c - 2) * 128:(c - 1) * 128, :], hn[:], reads=[rg("hn")], writes=[rg("OUT")], is_output=True)
                for _a in tiles:
                    _moe_tile(*_a)
                S.barrier()

        for l in layers:
            last = (l == 1)
            seq = [("setup", lambda: layer_setup(l)), ("n1", lambda: sweep_n1(l)), ("kv", lambda: sweep_kv(l)), ("hgrnA", lambda: sweep_hgrn(l, False))]
            if mode != "A":
                seq += [("exchange", lambda: exchange(l)), ("hgrnB", lambda: sweep_hgrn(l, True)), ("conv", lambda: sweep_conv(l)),
                        ("att", lambda: sweep_att(l)), ("merge", lambda: sweep_merge(l)), ("moe", lambda: sweep_moe(l, last))]
            for nm, fn in seq:
                fn()
                if stop == nm:
                    break
        if dbg is not None:
            srcs = {"H1": H1, "UT": UT, "YB": YB, "PA": PA, "YC": YC, "HL": HL, "KG": KG, "OF": OF, "KC": KC, "VC": VC, "SX": SX, "KX": KX, "VX": VX}
            for (dn, dap) in dbg_outs:
                S.dma("sp", dap, srcs[dn], reads=[rg(k) for k in list(R.keys())], is_output=True)
        S.emit()
    return nc, in_names


def _consts():
    c = np.zeros((128, 8, 128), np.float32)
    c[:, 0, :] = np.eye(128)
    s = np.arange(128)[:, None]
    t = np.arange(128)[None, :]
    same = (s // 32) == (t // 32)
    c[:, 1, :] = 0.0
    c[:, 2, :] = 0.0
    c[:, 3, :] = (same & (s <= t)) * 1.0
    c[:, 4, :] = (same & (s >= t)) * 1.0
    c[:, 5, :] = 1.0
    rm = np.zeros((64, 64), np.float32)
    for base in (0, 32):
        for d in range(16):
            rm[base + d + 16, base + d] = -1.0
            rm[base + d, base + d + 16] = 1.0
    c[0:64, 6, 0:64] = rm
    for q_ in range(4):
        c[:, 7, q_] = ((np.arange(128) // 32) == q_) * 1.0
        c[:, 7, 8 + q_] = ((np.arange(128) // 32) == q_) * 1.0
    c[:, 7, 4] = 1.0
    return c


def _rope_tables(L):
    GRID_W = 64
    pos = np.arange(L)
    row = (pos // GRID_W).astype(np.float32)
    col = (pos % GRID_W).astype(np.float32)
    inv = (np.float32(10000.0) ** (-np.arange(0, 32, 2, dtype=np.float32) / np.float32(32))).astype(np.float32)
    ar = row[:, None] * inv
    ac = col[:, None] * inv
    ang = np.concatenate([ar, ar, ac, ac], axis=-1)
    return np.cos(ang).astype(np.float32).T.copy(), np.sin(ang).astype(np.float32).T.copy()


def make_in_maps(inp, T, layer=None):
    B = inp["x"].shape[0]
    L = inp["x"].shape[1]
    nr = L // T
    assert B * nr == 8 and nr == 4
    cosL, sinL = _rope_tables(L)
    cst = _consts()
    f = lambda a: np.ascontiguousarray(np.asarray(a, dtype=np.float32))
    shared = {
        "w_mod": f(inp["w_mod"]),
        "b_modT": f(np.asarray(inp["b_mod"]).reshape(2, 48, 128).transpose(0, 2, 1)),
        "n1g": f(np.asarray(inp["norm1_g"]).reshape(2, 8, 128).transpose(0, 2, 1)),
        "n2g": f(np.asarray(inp["norm2_g"]).reshape(2, 8, 128).transpose(0, 2, 1)),
        "w_in": f(inp["w_in"]),
        "conv_w": f(np.asarray(inp["conv_w"]).reshape(2, 3, 4, 128).transpose(0, 3, 2, 1)),
        "lbp": f(np.asarray(inp["lb_param"]).reshape(2, 2, 4, 128).transpose(3, 0, 1, 2)),
        "hng": f(np.asarray(inp["hgrn_norm_g"]).reshape(2, 4, 128).transpose(0, 2, 1)),
        "qng": f(np.asarray(inp["q_norm_g"]).reshape(2, 64, 1)),
        "kng": f(np.asarray(inp["k_norm_g"]).reshape(2, 64, 1)),
        "w_a_out": f(inp["w_a_out"]), "w_b_out": f(inp["w_b_out"]), "w_c_out": f(inp["w_c_out"]), "w_o": f(inp["w_o"]),
        "router_w": f(inp["router_w"]),
        "rb": f(np.broadcast_to(np.asarray(inp["router_b"]).reshape(1, 16), (128, 16))),
        "w_gate": f(inp["w_gate"]), "w_up": f(inp["w_up"]), "w_down": f(inp["w_down"]),
        "fgc": f(np.asarray(inp["final_g"]).reshape(8, 128).T),
        "cst": cst,
    }
    if layer is not None:
        for k_ in ("w_mod", "b_modT", "n1g", "n2g", "w_in", "conv_w", "hng", "qng", "kng", "w_a_out", "w_b_out", "w_c_out", "w_o", "w_gate", "w_up", "w_down"):
            shared[k_] = np.ascontiguousarray(shared[k_][layer:layer + 1])
    maps = []
    for core in range(8):
        b, j = core // 4, core % 4
        m = dict(shared)
        m["x"] = f(inp["x"][b, j * T:(j + 1) * T])
        m["ctx"] = f(inp["ctx"][b])
        ccv = np.stack([np.asarray(inp["c"])[b].reshape(8, 128).T, np.asarray(inp["c_ctx"]).reshape(8, 128).T], axis=-1)
        m["cc"] = f(ccv)
        mk = np.zeros((128, 16), np.float32)
        for r in range(4):
            mk[:, r] = 1.0 if r < j else 0.0
            mk[:, 4 + r] = 1.0 if r > j else 0.0
            mk[:, 8 + r] = 1.0 if r == j - 1 else 0.0
            mk[:, 12 + r] = 1.0 if r == j + 1 else 0.0
        m["msk"] = mk
        m["cosT"] = f(cosL[:, j * T:(j + 1) * T])
        m["sinT"] = f(sinL[:, j * T:(j + 1) * T])
        maps.append(m)
    return maps


_NC_CACHE = {}


def _get(T, mode, layer):
    key = (T, mode, layer)
    if key not in _NC_CACHE:
        _NC_CACHE[key] = build(T, mode=mode, layer=layer)
    return _NC_CACHE[key]


def _run(prog, maps):
    nc, names = prog
    fm = [{k: m[k] for k in names} for m in maps]
    return run_bass_kernel_spmd(nc, fm, core_ids=list(range(8))).results


def kernel(**inputs):
    T = inputs["x"].shape[1] // 4
    B = inputs["x"].shape[0]
    outs = np.zeros((B, 4 * T, D), np.float32)
    hx = None
    for l in range(2):
        maps = make_in_maps(inputs, T, layer=l)
        if hx is not None:
            for core in range(8):
                maps[core]["x"], maps[core]["ctx"] = hx[core]
        ra = _run(_get(T, "A", l), maps)
        for core in range(8):
            g = (core // 4) * 4
            maps[core]["KG"] = np.ascontiguousarray(np.concatenate([ra[g + r]["KX"] for r in range(4)], axis=0))
            maps[core]["VG"] = np.ascontiguousarray(np.concatenate([ra[g + r]["VX"] for r in range(4)], axis=0))
            maps[core]["SG"] = np.ascontiguousarray(np.concatenate([ra[g + r]["SX"] for r in range(4)], axis=0))
        rb = _run(_get(T, "B", l), maps)
        for core in range(8):
            if l == 0:
                if hx is None:
                    hx = [None] * 8
                hx[core] = (np.ascontiguousarray(rb[core]["HL"]), np.ascontiguousarray(rb[core]["HC"]))
            else:
                b, j = core // 4, core % 4
                outs[b, j * T:(j + 1) * T] = rb[core]["out"]
    return outs
```
